# Optimizing a Trainium2 kernel written in Bass

```python
import jax, jax.numpy as jnp
from jax import lax
import numpy as np

D_MODEL = 1024
BATCH = 16
SEQ = 2048
DEPTH = 1

CHUNK = 128
SGU_WIDTH = 1024
SGU_GROUPS = 8
SGU_GROUP_DIM = SGU_WIDTH // SGU_GROUPS
ATT_HEADS = 8
ATT_HEAD_DIM = 128
ATT_WIDTH = ATT_HEADS * ATT_HEAD_DIM
DILATED_PATTERNS = ((128, 1), (512, 4), (2048, 16))
IN_WIDTH = 2 * SGU_WIDTH + 3 * ATT_WIDTH + 2 * D_MODEL
N_GROUPS = 4
EXPERTS_PER_GROUP = 4
N_EXPERTS = N_GROUPS * EXPERTS_PER_GROUP
TOP_K_EXPERTS = 2
D_EXPERT = 512
EPS = 1e-6
NEG_INF = -1e30

kernel_name = "hybrid_gmlp_dilated_attn_hmoe_block"


def _rmsnorm(x, g):
    xf = x.astype(jnp.float32)
    y = xf * lax.rsqrt(jnp.mean(xf * xf, axis=-1, keepdims=True) + EPS)
    return (y * g.astype(jnp.float32)).astype(x.dtype)


def _modulate(h, shift, scale):
    return h * (1 + scale[:, None, :]) + shift[:, None, :]


def _spatial_gating(u, v, norm_g, w_s, b_s):
    B, S, _ = v.shape
    v = _rmsnorm(v, norm_g)
    vc = v.reshape(B, S // CHUNK, CHUNK, SGU_GROUPS, SGU_GROUP_DIM)
    causal = jnp.tril(jnp.ones((CHUNK, CHUNK), dtype=bool))
    w = jnp.where(causal[None], w_s, 0)
    mixed = jnp.einsum('gts,bcsgk->bctgk', w, vc) + b_s.T[None, None, :, :, None]
    return u * mixed.reshape(B, S, SGU_WIDTH)


def _dilated_window_attention(q, k, v, window, dilation):
    B, S, H, Dh = q.shape
    L = window // dilation
    span = dilation * L
    Sp = -(-S // span) * span
    n = Sp // dilation
    nb = n // L

    def to_blocks(t):
        t = jnp.pad(t, ((0, 0), (0, Sp - S), (0, 0), (0, 0)))
        t = t.reshape(B, n, dilation, H, Dh).transpose(0, 2, 3, 1, 4)
        return t.reshape(B, dilation, H, nb, L, Dh)

    qb, kb, vb = to_blocks(q), to_blocks(k), to_blocks(v)

    def with_prev(t):
        prev = jnp.pad(t[:, :, :, :-1], ((0, 0), (0, 0), (0, 0), (1, 0), (0, 0), (0, 0)))
        return jnp.concatenate([prev, t], axis=4)

    k2, v2 = with_prev(kb), with_prev(vb)
    a = jnp.arange(L)[:, None]
    b = jnp.arange(2 * L)[None, :]
    band = (b >= a) & (b <= a + L)
    not_first = (jnp.arange(nb) > 0)[:, None, None]
    mask = band[None] & (not_first | (b >= L)[None])

    s = jnp.einsum('bdhnqe,bdhnke->bdhnqk', qb, k2) * (Dh ** -0.5)
    s = jnp.where(mask, s, NEG_INF)
    m = jnp.max(s, axis=-1, keepdims=True)
    p = jnp.exp(s - m)
    den = jnp.sum(p, axis=-1, keepdims=True)
    o = jnp.einsum('bdhnqk,bdhnke->bdhnqe', p, v2) / den
    lse = m + jnp.log(den)

    def from_blocks(t):
        last = t.shape[-1]
        t = t.reshape(B, dilation, H, n, last).transpose(0, 3, 1, 2, 4)
        return t.reshape(B, Sp, H, last)[:, :S]

    return from_blocks(o), from_blocks(lse)[..., 0]


def _mixer(h, w_in, sgu_norm_g, sgu_w, sgu_b, q_norm_g, k_norm_g, w_proj_a, w_proj_b, w_out):
    B, S, _ = h.shape
    proj = h @ w_in
    splits = np.cumsum([SGU_WIDTH, SGU_WIDTH, ATT_WIDTH, ATT_WIDTH, ATT_WIDTH, D_MODEL]).tolist()
    u, v_sgu, q, k, v_att, g_a, g_b = jnp.split(proj, splits, axis=-1)

    y_a = _spatial_gating(jax.nn.gelu(u), jax.nn.gelu(v_sgu), sgu_norm_g, sgu_w, sgu_b) @ w_proj_a

    q = _rmsnorm(q.reshape(B, S, ATT_HEADS, ATT_HEAD_DIM), q_norm_g).astype(jnp.float32)
    k = _rmsnorm(k.reshape(B, S, ATT_HEADS, ATT_HEAD_DIM), k_norm_g).astype(jnp.float32)
    v_att = v_att.reshape(B, S, ATT_HEADS, ATT_HEAD_DIM).astype(jnp.float32)
    outs, lses = [], []
    for window, dilation in DILATED_PATTERNS:
        o_p, lse_p = _dilated_window_attention(q, k, v_att, window, dilation)
        outs.append(o_p)
        lses.append(lse_p)
    alpha = jax.nn.softmax(jnp.stack(lses, axis=0), axis=0)
    o = jnp.einsum('pbsh,pbshe->bshe', alpha, jnp.stack(outs, axis=0))
    y_b = o.reshape(B, S, ATT_WIDTH).astype(h.dtype) @ w_proj_b

    merged = jax.nn.sigmoid(g_a) * y_a + jax.nn.sigmoid(g_b) * y_b
    return merged @ w_out


def _hierarchical_moe(h, w_rg, b_rg, w_re, b_re, w_gate, w_up, w_down):
    B, S, D = h.shape
    t = h.reshape(B * S, D)
    g_logits = (t @ w_rg + b_rg).astype(jnp.float32)
    g_prob = jax.nn.softmax(g_logits, axis=-1)
    g_top = jnp.argmax(g_logits, axis=-1)
    p_group = jnp.take_along_axis(g_prob, g_top[:, None], axis=-1)
    e_logits = (jnp.einsum('td,gde->tge', t, w_re) + b_re).astype(jnp.float32)
    e_logits = jnp.take_along_axis(e_logits, g_top[:, None, None], axis=1)[:, 0]
    top_val, top_idx = lax.top_k(e_logits, TOP_K_EXPERTS)
    p_exp = jax.nn.softmax(top_val, axis=-1) * p_group
    expert_ids = g_top[:, None] * EXPERTS_PER_GROUP + top_idx
    combine = jnp.sum(jax.nn.one_hot(expert_ids, N_EXPERTS, dtype=jnp.float32) * p_exp[..., None], axis=1)
    combine = combine.astype(t.dtype)
    y = jnp.zeros_like(t)
    for e in range(N_EXPERTS):
        he = jax.nn.silu(t @ w_gate[e]) * (t @ w_up[e])
        y = y + combine[:, e:e + 1] * (he @ w_down[e])
    return y.reshape(B, S, D)


def setup_inputs(seed: int = 0) -> dict:
    key = jax.random.key(seed)
    ks = jax.random.split(key, 24)
    f32 = jnp.float32

    def nrm(k, shape, scale):
        return jax.random.normal(k, shape, f32) * scale

    Dp = DEPTH
    return {
        "x": nrm(ks[0], (BATCH, SEQ, D_MODEL), 1.0),
        "c": nrm(ks[1], (BATCH, D_MODEL), 1.0),
        "w_ada": nrm(ks[2], (Dp, D_MODEL, 6 * D_MODEL), 0.5 * D_MODEL ** -0.5),
        "b_ada": nrm(ks[3], (Dp, 6 * D_MODEL), 0.02),
        "norm1_g": 1.0 + nrm(ks[4], (Dp, D_MODEL), 0.02),
        "w_in": nrm(ks[5], (Dp, D_MODEL, IN_WIDTH), D_MODEL ** -0.5),
        "sgu_norm_g": 1.0 + nrm(ks[6], (Dp, SGU_WIDTH), 0.02),
        "sgu_w": nrm(ks[7], (Dp, SGU_GROUPS, CHUNK, CHUNK), CHUNK ** -0.5),
        "sgu_b": 1.0 + nrm(ks[8], (Dp, SGU_GROUPS, CHUNK), 0.02),
        "q_norm_g": 1.0 + nrm(ks[9], (Dp, ATT_HEAD_DIM), 0.02),
        "k_norm_g": 1.0 + nrm(ks[10], (Dp, ATT_HEAD_DIM), 0.02),
        "w_proj_a": nrm(ks[11], (Dp, SGU_WIDTH, D_MODEL), SGU_WIDTH ** -0.5),
        "w_proj_b": nrm(ks[12], (Dp, ATT_WIDTH, D_MODEL), ATT_WIDTH ** -0.5),
        "w_out": nrm(ks[13], (Dp, D_MODEL, D_MODEL), D_MODEL ** -0.5),
        "norm2_g": 1.0 + nrm(ks[14], (Dp, D_MODEL), 0.02),
        "w_router_group": nrm(ks[15], (Dp, D_MODEL, N_GROUPS), D_MODEL ** -0.5),
        "b_router_group": nrm(ks[16], (Dp, N_GROUPS), 0.01),
        "w_router_expert": nrm(ks[17], (Dp, N_GROUPS, D_MODEL, EXPERTS_PER_GROUP), D_MODEL ** -0.5),
        "b_router_expert": nrm(ks[18], (Dp, N_GROUPS, EXPERTS_PER_GROUP), 0.01),
        "w_gate": nrm(ks[19], (Dp, N_EXPERTS, D_MODEL, D_EXPERT), D_MODEL ** -0.5),
        "w_up": nrm(ks[20], (Dp, N_EXPERTS, D_MODEL, D_EXPERT), D_MODEL ** -0.5),
        "w_down": nrm(ks[21], (Dp, N_EXPERTS, D_EXPERT, D_MODEL), D_EXPERT ** -0.5),
    }


def reference(x, c, w_ada, b_ada, norm1_g, w_in, sgu_norm_g, sgu_w, sgu_b, q_norm_g, k_norm_g,
              w_proj_a, w_proj_b, w_out, norm2_g, w_router_group, b_router_group,
              w_router_expert, b_router_expert, w_gate, w_up, w_down):
    cond = jax.nn.silu(c)
    for l in range(DEPTH):
        mod = cond @ w_ada[l] + b_ada[l]
        shift1, scale1, gate1, shift2, scale2, gate2 = jnp.split(mod, 6, axis=-1)
        h = _modulate(_rmsnorm(x, norm1_g[l]), shift1, scale1)
        y = _mixer(h, w_in[l], sgu_norm_g[l], sgu_w[l], sgu_b[l], q_norm_g[l], k_norm_g[l],
                   w_proj_a[l], w_proj_b[l], w_out[l])
        x = x + gate1[:, None, :] * y
        h2 = _modulate(_rmsnorm(x, norm2_g[l]), shift2, scale2)
        y2 = _hierarchical_moe(h2, w_router_group[l], b_router_group[l], w_router_expert[l],
                               b_router_expert[l], w_gate[l], w_up[l], w_down[l])
        x = x + gate2[:, None, :] * y2
    return x
```

```python
import contextlib
import numpy as np
import ml_dtypes
import concourse.bass as bass
import concourse.mybir as mybir
from concourse.bass_utils import run_bass_kernel_spmd

F32 = mybir.dt.float32
BF16 = mybir.dt.bfloat16
AF = mybir.ActivationFunctionType
ALU = mybir.AluOpType
AX = mybir.AxisListType

NSEQ = 2
_STOP = 0
S = 2048
D = 1024
EPS = 1e-6
N_EXP = 16


class Prog:
    ENGS = ("pe", "act", "dve", "pool", "sp")

    def __init__(self, nc):
        self.nc = nc
        self.ops = {e: [] for e in self.ENGS}
        self.cnt = {}
        self.seen = {e: {} for e in self.ENGS}
        self.res = {}
        self.sems = {}
        self.final = []
        self.cond_stack = []
        self.cond_first = {}
        self.ncond = 0

    def cond_begin(self, flag_ap, flag_res):
        self.ncond += 1
        self.cond_stack.append((self.ncond, flag_ap, flag_res))

    def cond_end(self):
        self.cond_stack.pop()

    def _deps(self, eng, reads, writes):
        deps = {}

        def addall(d):
            for s, v in d.items():
                if deps.get(s, 0) < v:
                    deps[s] = v
        for r in reads:
            st = self.res.get(r)
            if st is not None:
                addall(st[0])
        for w in writes:
            st = self.res.get(w)
            if st is not None:
                addall(st[0])
                addall(st[1])
        waits = []
        for s, v in deps.items():
            if s == eng and eng == "pe":
                continue
            if self.seen[eng].get(s, 0) >= v:
                continue
            self.seen[eng][s] = v
            waits.append((s, v))
        return waits

    def _commit(self, ev, reads, writes):
        s, v = ev
        for r in reads:
            st = self.res.setdefault(r, [{}, {}])
            st[1][s] = max(st[1].get(s, 0), v)
        for w in writes:
            st = self.res.setdefault(w, [{}, {}])
            st[0][s] = max(st[0].get(s, 0), v)

    def alias(self, new, old):
        ev = {}
        for o in old:
            st = self.res.get(o)
            if st is None:
                continue
            for d in st:
                for s, v in d.items():
                    ev[s] = max(ev.get(s, 0), v)
        for n in new:
            st = self.res.setdefault(n, [{}, {}])
            for s, v in ev.items():
                st[0][s] = max(st[0].get(s, 0), v)

    def op(self, eng, fn, reads=(), writes=()):
        cond = None
        if self.cond_stack:
            for (cid, flag_ap, flag_res) in self.cond_stack:
                if (cid, eng) not in self.cond_first:
                    self.cond_first[(cid, eng)] = self._deps(eng, [flag_res], [])
            saved = dict(self.seen[eng])
            waits = self._deps(eng, reads, writes)
            self.seen[eng] = saved
            cond = tuple((cid, flag_ap) for (cid, flag_ap, _) in self.cond_stack)
        else:
            waits = self._deps(eng, reads, writes)
        self.cnt[eng] = self.cnt.get(eng, 0) + 1
        ev = (eng, self.cnt[eng])
        self.ops[eng].append((waits, fn, eng, 1, cond, self.cnt[eng]))
        self._commit(ev, reads, writes)
        return ev

    def dma(self, q, sem, fn, reads=(), writes=(), final=False):
        waits = self._deps(q, reads, writes)
        self.cnt[sem] = self.cnt.get(sem, 0) + 16
        ev = (sem, self.cnt[sem])
        self.ops[q].append((waits, fn, sem, 16, None, self.cnt[sem]))
        self._commit(ev, reads, writes)
        if final:
            self.final.append(ev)
        return ev

    def emit(self):
        nc = self.nc
        keys = list(self.cnt.keys())
        with contextlib.ExitStack() as es:
            for k in keys:
                self.sems[k] = es.enter_context(nc.semaphore("s_" + str(k)))
            block = es.enter_context(nc.Block())
            sems = self.sems

            regs = {"pe": [es.enter_context(nc.tensor.register("rg_pe%d" % i)) for i in range(2)],
                    "act": [es.enter_context(nc.scalar.register("rg_act%d" % i)) for i in range(2)],
                    "dve": [es.enter_context(nc.vector.register("rg_dve%d" % i)) for i in range(2)]}

            def emit_one(e, item):
                waits, fn, semk, inc, _, _c = item
                for s, v in waits:
                    e.wait_ge(sems[s], v)
                fn(e).then_inc(sems[semk], inc)

            def emit_range(e, engname, ops, i, j, depth):
                k = i
                while k < j:
                    chain = ops[k][4] or ()
                    if len(chain) <= depth:
                        emit_one(e, ops[k])
                        k += 1
                        continue
                    cid, ap = chain[depth]
                    m = k
                    while m < j and ops[m][4] is not None and len(ops[m][4]) > depth and ops[m][4][depth][0] == cid:
                        m += 1
                    for s, v in self.cond_first[(cid, engname)]:
                        e.wait_ge(sems[s], v)
                    r = regs[engname][depth]
                    e.reg_load(r, ap)
                    with e.If_ne(r, 0):
                        emit_range(e, engname, ops, k, m, depth + 1)
                    with e.Else():
                        e.wait_ge(sems[engname], ops[k][5] - 1)
                        e.sem_inc(sems[engname], m - k)
                    k = m

            def replay(engname, e):
                ops = self.ops[engname]
                emit_range(e, engname, ops, 0, len(ops), 0)
                if engname == "sp":
                    fin = {}
                    for s, v in self.final:
                        fin[s] = max(fin.get(s, 0), v)
                    for s, v in fin.items():
                        e.wait_ge(sems[s], v)

            @block.sync
            def _(e):
                replay("sp", e)

            @block.tensor
            def _(e):
                replay("pe", e)

            @block.scalar
            def _(e):
                replay("act", e)

            @block.vector
            def _(e):
                replay("dve", e)

            @block.gpsimd
            def _(e):
                replay("pool", e)


def tokslice(p, b):
    if p == 0:
        return slice(b * 128, (b + 1) * 128)
    if p == 1:
        r, blk = b // 4, b % 4
        st = r + 512 * blk
        return slice(st, st + 4 * 127 + 1, 4)
    return slice(b, b + 16 * 127 + 1, 16)


def build(nseq=NSEQ, dbg=()):
    nc = bass.Bass("TRN2", target_bir_lowering=False)
    P = Prog(nc)

    def din(name, shape, dt=F32):
        return nc.dram_tensor(name, shape, dt, kind="ExternalInput").ap()
    x_d = din("x", [NSEQ, S, D]); c_d = din("c", [NSEQ, D])
    w_ada = din("w_ada", [D, 6 * D]); b_ada = din("b_ada", [6 * D]); n1g = din("norm1_g", [D])
    w_in = din("w_in", [D, 7 * D]); sgu_g = din("sgu_norm_g", [D]); sgu_wT = din("sgu_wT", [8, 128, 128])
    sgu_b = din("sgu_b", [8 * 128]); qg = din("q_norm_g", [128]); kg = din("k_norm_g", [128])
    w_pa = din("w_proj_a", [D, D]); w_pb = din("w_proj_b", [D, D]); w_out = din("w_out", [D, D])
    n2g = din("norm2_g", [D]); w_r = din("w_r", [D, 20]); b_r = din("b_r", [20])
    w_gate = din("w_gate", [N_EXP, D, 512]); w_up = din("w_up", [N_EXP, D, 512]); w_down = din("w_down", [N_EXP, 512, D])
    identb_d = din("identb", [128, 128], BF16); identf_d = din("identf", [128, 128])
    trilT_d = din("trilT", [128, 128]); amask_d = din("amask", [128, 3, 512], BF16)
    utri_d = din("utri", [128, 128], BF16); blkc_d = din("blkc", [128, 2, 8])
    y_d = nc.dram_tensor("y", [NSEQ, S, D], F32, kind="ExternalOutput").ap()
    X1_d = [nc.dram_tensor("scr_x1%d" % i, [S, D], F32, kind="Internal").ap() for i in range(NSEQ)]
    CW_d = [nc.dram_tensor("scr_cw%d" % i, [S, 17], F32, kind="Internal").ap() for i in range(NSEQ)]
    Y_d = [nc.dram_tensor("scr_y%d" % i, [S, D], F32, kind="Internal").ap() for i in range(NSEQ)]
    dbg_out = {}

    with contextlib.ExitStack() as es:
        def sb(name, shape, dt=F32):
            return es.enter_context(nc.sbuf_tensor("sb_" + name, shape, dt))

        def ps(name, shape, dt=F32):
            return es.enter_context(nc.psum_tensor("ps_" + name, shape, dt))
        R01 = sb("R01", [128, 32768], BF16)
        R2 = sb("R2", [128, 16384], BF16)
        R4 = sb("R4", [128, 14336], BF16)
        Wt = sb("Wt", [128, 6 * 4096], BF16)
        identb = sb("identb", [128, 128], BF16); identf = sb("identf", [128, 128])
        onesb = sb("onesb", [128, 128], BF16); amask = sb("amask", [128, 3, 512], BF16)
        vecs = sb("vecs", [80, 128]); colsT = sb("colsT", [128, 80]); condb = sb("condb", [128, 16], BF16)
        modc = sb("modc", [128, 48, 2]); Gc = sb("Gc", [128, 2, 2, 8])
        gaterow = sb("gaterow", [128, 2, 2, 1024])
        gsgu = sb("gsgu", [128, 1024]); WsT = sb("WsT", [128, 8, 128], BF16)
        bs2 = sb("bs2", [33, 1024], BF16); gqk = sb("gqk", [128, 2]); epsT = sb("epsT", [128, 1]); ones1 = sb("ones1", [128, 1])
        Wr = sb("Wr", [128, 8, 20], BF16); brow = sb("brow", [128, 20])
        logits = sb("logits", [128, 16, 20]); cw = sb("cw", [128, 16, 17])
        junk = sb("junk", [128, 1024], BF16)
        I32 = mybir.dt.int32
        utri = sb("utri", [128, 128], BF16); blkc = sb("blkc", [128, 2, 8]); ohb = sb("ohb", [128, 16, 4], BF16)
        tot = sb("tot", [128, 4]); stt = sb("stt", [128, 4]); ent = sb("ent", [128, 4]); cnt3 = sb("cnt3", [128, 16, 4])
        posf = sb("posf", [128, 16]); posi = sb("posi", [128, 16], I32)
        flA = sb("flA", [128, 4, 8]); flB = sb("flB", [128, 4, 8]); flags_i = sb("flags_i", [128, 32], I32)
        flQ = sb("flQ", [128, 4, 2]); flags_q = sb("flags_q", [128, 8], I32)
        ssA = sb("ssA", [128, 16]); lnA = sb("lnA", [128, 16]); rsA = sb("rsA", [128, 16])
        ssV = sb("ssV", [128, 4]); lnV = sb("lnV", [128, 4]); rsV = sb("rsV", [128, 4])
        psA = [ps("psA0", [128, 512]), ps("psA1", [128, 512])]
        psB = [ps("psB0", [128, 512]), ps("psB1", [128, 512])]
        psT = [ps("psT0", [128, 8, 128], BF16), ps("psT1", [128, 8, 128], BF16)]
        psS = ps("psS", [128, 512]); psO = ps("psO", [128, 512])

        def view(reg, off, shape, dt):
            n = int(np.prod(shape[1:]))
            nb = n * (4 if dt == F32 else 2)
            ap = reg[:, off // 2: (off + nb) // 2]
            if dt == F32:
                ap = ap.bitcast(F32)
            if len(shape) == 3:
                ap = ap.rearrange("p (a b) -> p a b", b=shape[2])
            elif len(shape) == 4:
                ap = ap.rearrange("p (a b c) -> p a b c", b=shape[2], c=shape[3])
            return ap
        rt = {}
        _o = 16384
        for n, sh in [("gmax", []), ("ohg", [4]), ("dg", [4]), ("eg", [4]), ("sumg", []), ("pg", []), ("tmp", [4, 4]),
                      ("sel", [4]), ("m1", []), ("oh1", [4]), ("sel2", [4]), ("m2", []), ("oh2", [4]), ("d", []),
                      ("ed", []), ("den", []), ("r", []), ("p1", []), ("p2", []), ("c1", [4]), ("c2", [4]), ("cwg", [4])]:
            shape = [128, 16] + sh
            rt[n] = view(R4, _o, shape, F32)
            _o += int(np.prod(shape[1:])) * 4
        RTN = ["rt_" + n for n in rt]
        K32 = 32768
        hT = view(R01, 0, [128, 8, S], BF16)
        oT = view(R01, K32, [128, 8, S], BF16)
        zT = oT
        acc = view(R01, 0, [128, 16, D], F32)
        mg = view(R2, 0, [128, 8, S], BF16)
        h2T = mg

        def wslot(i, shape):
            return view(Wt, i * 8192, shape, BF16)

        def wres(i, n=1):
            return [("W", (i + j) % 6) for j in range(n)]

        def mm(out, pairs, reads, writes):
            def fn(e, out=out, pairs=pairs):
                n = len(pairs)
                for i, (l, r) in enumerate(pairs):
                    ins = e.matmul(out, lhsT=l, rhs=r, start=(i == 0), stop=(i == n - 1))
                return ins
            P.op("pe", fn, reads, writes)

        def add_dbg(name, ap, shape, reads):
            if name in dbg:
                d = nc.dram_tensor("dbg_" + name, shape, ap.dtype, kind="ExternalOutput").ap()
                P.dma("sp", "dbg_" + name, lambda e, d=d, ap=ap: e.dma_start(out=d, in_=ap), reads=reads, final=True)

        cl = []

        def cdma(out, in_, res, q="sp"):
            P.dma(q, "const" if q == "sp" else "constp", lambda e, out=out, in_=in_: e.dma_start(out=out, in_=in_), writes=[res])
            if q == "sp":
                cl.append(res)
        cdma(identb[:], identb_d, "identb"); cdma(identf[:], identf_d, "identf"); cdma(amask[:], amask_d, "amask")
        cdma(vecs[0:8, :], n1g.rearrange("(k p) -> k p", p=128), "vecs")
        cdma(vecs[8:16, :], n2g.rearrange("(k p) -> k p", p=128), "vecs")
        cdma(vecs[16:64, :], b_ada.rearrange("(k p) -> k p", p=128), "vecs")
        cdma(vecs[64:80, :], c_d.rearrange("s (k p) -> (s k) p", p=128), "vecs")
        cdma(gqk[:, 0:1], qg.rearrange("(p o) -> p o", o=1), "gqk")
        cdma(gqk[:, 1:2], kg.rearrange("(p o) -> p o", o=1), "gqk")
        cdma(gsgu[:], sgu_g.partition_broadcast(128), "gsgu")
        cdma(brow[:], b_r.partition_broadcast(128), "brow")
        gbias = view(R4, 0, [128, 2, 1024], F32)
        cdma(gbias[:, 0, :], b_ada[2 * D:3 * D].partition_broadcast(128), "gbias")
        cdma(gbias[:, 1, :], b_ada[5 * D:6 * D].partition_broadcast(128), "gbias")
        wsf = view(R4, 8192, [128, 8, 128], F32)
        trilT = view(R4, 12288, [128, 128], F32)
        bsf = view(R4, 12800, [128, 1024], F32)
        bsh = view(R4, 16896, [128, 1024], F32)
        rep = view(R4, 20992, [128, 2, 8, 128], BF16)
        cdma(wsf, sgu_wT.rearrange("g s t -> s g t"), "wsf")
        cdma(trilT, trilT_d, "trilT")
        cdma(bsf[0:1, :], sgu_b.rearrange("(o n) -> o n", o=1), "bsf")
        cdma(bsf[32:33, :], sgu_b.rearrange("(o n) -> o n", o=1), "bsf")
        cdma(Wr[:], w_r.rearrange("(k p) n -> p k n", p=128), "Wr", q="pool")
        cdma(utri[:], utri_d, "utri"); cdma(blkc[:], blkc_d, "blkc")
        for r in set(cl):
            P.res[r][0]["const"] = P.cnt["const"]

        P.op("dve", lambda e: e.memset(onesb[:], 1.0), writes=["onesb"])
        P.op("dve", lambda e: e.memset(epsT[:], EPS), writes=["epsT"])
        P.op("dve", lambda e: e.memset(ones1[:], 1.0), writes=["ones1"])
        P.op("dve", lambda e: e.memset(bs2[:], 0.0), writes=["bs2"])
        P.op("dve", lambda e: e.tensor_tensor(out=WsT[:], in0=wsf, in1=trilT.unsqueeze(1).to_broadcast([128, 8, 128]), op=ALU.mult),
             reads=["wsf", "trilT"], writes=["WsT"])
        P.op("dve", lambda e: e.tensor_copy(out=bs2[0:1, :], in_=bsf[0:1, :]), reads=["bsf", "bs2"], writes=["bs2a"])
        P.op("dve", lambda e: e.tensor_copy(out=junk[32:33, :], in_=bsf[32:33, :]), reads=["bsf"], writes=["junk"])
        P.op("dve", lambda e: e.tensor_copy(out=bsh[32:33, :], in_=junk[32:33, :]), reads=["junk"], writes=["bsh"])
        P.op("dve", lambda e: e.tensor_tensor(out=bsh[32:33, :], in0=bsf[32:33, :], in1=bsh[32:33, :], op=ALU.subtract),
             reads=["bsh", "bsf"], writes=["bsh"])
        P.op("dve", lambda e: e.tensor_copy(out=bs2[32:33, :], in_=bsh[32:33, :]), reads=["bsh", "bs2"], writes=["bs2b"])
        BS2 = ["bs2a", "bs2b"]
        P.op("dve", lambda e: e.tensor_scalar(out=gqk[:, 0:1], in0=gqk[:, 0:1], scalar1=float(128 ** -0.5), scalar2=None, op0=ALU.mult),
             reads=["gqk"], writes=["gqk"])
        P.op("pe", lambda e: e.transpose(psA[0][:, 0:80], vecs[0:80, :], identf[0:80, 0:80]), reads=["vecs", "identf"], writes=["psA0"])
        P.op("dve", lambda e: e.tensor_copy(out=colsT[:], in_=psA[0][:, 0:80]), reads=["psA0"], writes=["colsT"])
        P.op("act", lambda e: e.activation(out=condb[:], in_=colsT[:, 64:80], func=AF.Silu), reads=["colsT"], writes=["condb"])
        for s in range(2):
            P.op("dve", lambda e, s=s: e.tensor_copy(out=rep[:, s, :, :], in_=condb[:, s * 8:(s + 1) * 8].unsqueeze(2).to_broadcast([128, 8, 128])),
                 reads=["condb"], writes=["rep"])
        for j in range(6):
            sl = 2 * (j % 3)
            Wp = wslot(sl, [128, 8, 1024])
            P.dma("pool", "w%d" % sl, lambda e, Wp=Wp, j=j: e.dma_start(out=Wp, in_=w_ada[:, j * D:(j + 1) * D].rearrange("(k p) n -> p k n", p=128)),
                  writes=wres(sl, 2))
            for m in range(8):
                mm(psB[0][:, (j * 8 + m) * 2:(j * 8 + m) * 2 + 2],
                   [(Wp[:, k, m * 128:(m + 1) * 128], condb[:, k:16:8]) for k in range(8)],
                   reads=wres(sl, 2) + ["condb"], writes=["psB0"])
            if j in (2, 5):
                for s in range(2):
                    for half in range(2):
                        mm(psA[half][:], [(rep[:, s, k, :], Wp[:, k, half * 512:(half + 1) * 512]) for k in range(8)],
                           reads=wres(sl, 2) + ["rep"], writes=["psA%d" % half])
                        P.op("dve", lambda e, s=s, half=half, j=j: e.tensor_tensor(
                            out=gaterow[:, s, j // 3, half * 512:(half + 1) * 512], in0=psA[half][:],
                            in1=gbias[:, j // 3, half * 512:(half + 1) * 512], op=ALU.add),
                            reads=["psA%d" % half, "gbias"], writes=["gaterow"])
        P.op("dve", lambda e: e.tensor_tensor(out=modc[:], in0=psB[0][:, 0:96].rearrange("p (a b) -> p a b", b=2),
                                              in1=colsT[:, 16:64].unsqueeze(2).to_broadcast([128, 48, 2]), op=ALU.add),
             reads=["psB0", "colsT"], writes=["modc"])
        for wh in range(2):
            for s in range(2):
                P.op("dve", lambda e, wh=wh, s=s: e.scalar_tensor_tensor(
                    out=Gc[:, wh, s, :], in0=modc[:, (1 + 3 * wh) * 8:(2 + 3 * wh) * 8, s], scalar=1.0,
                    in1=colsT[:, wh * 8:(wh + 1) * 8], op0=ALU.add, op1=ALU.mult), reads=["modc", "colsT"], writes=["Gc"])

        def shiftc(wh, s, k):
            return modc[:, 3 * wh * 8 + k, s:s + 1]

        R4A = ["xs0", "xs1", "xs2", "xnb0", "xnb1"]
        R4P = ["gbias", "wsf", "trilT", "bsf", "bsh", "rep"]
        R4B = ["accB", "lnt0", "lnt1", "rs0", "rs1"]
        R2B = ["qT", "kT", "vT", "vtok", "sq0", "sq1", "pT0", "pT1", "pT2"]
        R4C = ["gv0", "gv1", "gv2", "gv3", "vn", "ug0", "ug1"]
        R4D = ["heT0", "heT1", "sgD0", "sgD1", "stg0", "stg1"]
        xs = [view(R4, i * 4096, [128, 1024], F32) for i in range(3)]
        xnb = [view(R4, 12288 + i * 2048, [128, 1024], BF16) for i in range(2)]

        def rstd_small(ss_ap, ln_ap, rs_ap, scale, r, w):
            P.op("act", lambda e: e.activation(out=ln_ap, in_=ss_ap, func=AF.Ln, scale=scale, bias=epsT[:]), reads=r + ["epsT"], writes=[w + "_ln"])
            P.op("act", lambda e: e.activation(out=rs_ap, in_=ln_ap, func=AF.Exp, scale=-0.5), reads=[w + "_ln"], writes=[w])

        def norm_transpose(src_ap, src_res, tt, wh, s, dstT, dst_res, extra_w=(), defer=False):
            P.op("act", lambda e: e.activation(out=junk[:], in_=src_ap, func=AF.Square, accum_out=ssA[:, tt:tt + 1]),
                 reads=src_res, writes=["junk", ("ssA", tt)])
            rstd_small(ssA[:, tt:tt + 1], lnA[:, tt:tt + 1], rsA[:, tt:tt + 1], 1.0 / D, [("ssA", tt)], "rsA%d" % tt)
            xb = xnb[tt % 2]
            P.op("dve", lambda e: e.tensor_scalar(out=xb, in0=src_ap, scalar1=rsA[:, tt:tt + 1], scalar2=None, op0=ALU.mult),
                 reads=src_res + ["rsA%d" % tt], writes=["xnb%d" % (tt % 2)])
            if not defer:
                transpose_evac(tt, wh, s, dstT, dst_res, extra_w)

        def transpose_evac(tt, wh, s, dstT, dst_res, extra_w=()):
            xb = xnb[tt % 2]
            pt = psT[tt % 2]

            def tr(e):
                for k in range(8):
                    ins = e.transpose(pt[:, k, :], xb[:, k * 128:(k + 1) * 128], identb[:])
                return ins
            P.op("pe", tr, reads=["xnb%d" % (tt % 2), "identb"], writes=["psT%d" % (tt % 2)])

            def ev_act(e):
                for k in range(8):
                    ins = e.activation(out=dstT[:, k, tt * 128:(tt + 1) * 128], in_=pt[:, k, :], func=AF.Identity,
                                       scale=Gc[:, wh, s, k:k + 1], bias=shiftc(wh, s, k))
                return ins

            def ev_dve(e):
                for k in range(8):
                    ins = e.tensor_scalar(out=dstT[:, k, tt * 128:(tt + 1) * 128], in0=pt[:, k, :], scalar1=Gc[:, wh, s, k:k + 1],
                                          scalar2=shiftc(wh, s, k), op0=ALU.mult, op1=ALU.add)
                return ins
            if tt % 2 == 0:
                P.op("act", ev_act, reads=["psT%d" % (tt % 2), "Gc", "modc"], writes=[dst_res] + list(extra_w))
            else:
                P.op("dve", ev_dve, reads=["psT%d" % (tt % 2), "Gc", "modc"], writes=[dst_res] + list(extra_w))

        def seq_body(s, prevR4):
            P.alias(R4A, prevR4)
            P.alias([("hT", st) for st in range(4)], [("acc", tt, hh) for tt in range(16) for hh in range(2)])
            P.op("dve", lambda e: e.memset(ssA[:], 0.0), writes=[("ssA", i) for i in range(16)])
            for tt in range(16):
                sl = tt % 3
                P.dma("sp" if s == 0 else "act", "x%d" % sl, lambda e, sl=sl, tt=tt: e.dma_start(out=xs[sl], in_=x_d[s, tt * 128:(tt + 1) * 128, :]),
                      writes=["xs%d" % sl])
                norm_transpose(xs[sl], ["xs%d" % sl], tt, 0, s, hT, ("hT", tt // 4), defer=True)
                if tt >= 1:
                    transpose_evac(tt - 1, 0, s, hT, ("hT", (tt - 1) // 4))
            transpose_evac(15, 0, s, hT, ("hT", 3))
            add_dbg("hT%d" % s, hT, [128, 8, S], [("hT", st) for st in range(4)])

            P.alias(R4B, R4A + RTN + ["ot0", "ot1", "ot2"])
            P.alias(R2B, [("h2T", tt) for tt in range(16)] + R4D)
            P.alias([("oT", h) for h in range(8)], [("acc", tt, hh) for tt in range(16) for hh in range(2)])
            qT = view(R2, 0, [128, S], BF16); kT = view(R2, 4096, [128, S], BF16); vT = view(R2, 8192, [128, S], BF16)
            vtok = view(R2, 12288, [128, 3, 16, 128], BF16)
            pT = [view(R2, 24576 + i * 1024, [128, 512], BF16) for i in range(3)]
            sqb = [view(R2, 27648 + i * 1024, [128, 512], BF16) for i in range(2)]
            accB = view(R4, 0, [128, 2, S], F32)
            lnt = [view(R4, 16384 + i * 2048, [128, 512], F32) for i in range(2)]
            rs = [view(R4, 20480 + i * 2048, [128, 512], F32) for i in range(2)]
            Sb = [(psS, "psS"), (psB[0], "psB0")]
            Ob = [(psO, "psO"), (psB[1], "psB1")]

            def load_head(h):
                sl = (s * 8 + h) % 6
                wv = wslot(sl, [128, 8, 3, 128])
                for j in range(3):
                    c0 = (2 + j) * D + h * 128
                    P.dma("pool", "w%d" % sl, lambda e, j=j, c0=c0: e.dma_start(
                        out=wv[:, :, j, :], in_=w_in[:, c0:c0 + 128].rearrange("(k p) n -> p k n", p=128)),
                        writes=wres(sl))
                return sl, wv
            def load16(slot, src):
                Wv = wslot(slot, [128, 8, 1024])
                P.dma("pool", "w%d" % slot, lambda e: e.dma_start(out=Wv, in_=src.rearrange("(k p) n -> p k n", p=128)), writes=wres(slot, 2))
                return Wv
            c4spec = {"Wgb": (0, w_in[:, 6 * D:7 * D]), "Wpb": (2, w_pb), "Wvs": (4, w_in[:, D:2 * D])}
            c4w = {}
            nxh = load_head(0)
            ci = 0
            for h in range(8):
                sl, wv = nxh
                if h + 1 < 8:
                    nxh = load_head(h + 1)
                if h == 6:
                    busy = {(s * 8 + 6) % 6, (s * 8 + 7) % 6}
                    for nm_, (slot_, src_) in c4spec.items():
                        if not ({slot_, slot_ + 1} & busy):
                            c4w[nm_] = load16(slot_, src_)
                items = [(j, st, dst, dn) for (j, dst, dn) in ((1, kT, "kT"), (2, vT, "vT"), (0, qT, "qT")) for st in range(4)]

                def post2(a, j, st, dst, dn):
                    tsl = slice(st * 512, (st + 1) * 512)
                    mm(psB[a][:], [(onesb[:], sqb[a])], reads=["onesb", "sq%d" % a], writes=["psB%d" % a])
                    P.op("act", lambda e: e.activation(out=lnt[a], in_=psB[a][:], func=AF.Ln, scale=1.0 / 128, bias=epsT[:]),
                         reads=["psB%d" % a, "epsT"], writes=["lnt%d" % a])
                    P.op("act", lambda e: e.activation(out=rs[a], in_=lnt[a], func=AF.Exp, scale=-0.5), reads=["lnt%d" % a], writes=["rs%d" % a])
                    P.op("dve", lambda e: e.scalar_tensor_tensor(out=dst[:, tsl], in0=dst[:, tsl], scalar=gqk[:, j:j + 1], in1=rs[a],
                                                                  op0=ALU.mult, op1=ALU.mult),
                         reads=[dn, "gqk", "rs%d" % a], writes=[dn])
                pend = None
                for (j, st, dst, dn) in items:
                    a = ci % 2
                    ci += 1
                    tsl = slice(st * 512, (st + 1) * 512)
                    mm(psA[a][:], [(wv[:, k, j, :], hT[:, k, tsl]) for k in range(8)],
                       reads=wres(sl) + [("hT", st)], writes=["psA%d" % a])
                    P.op("dve", lambda e, a=a, dst=dst, tsl=tsl: e.tensor_copy(out=dst[:, tsl], in_=psA[a][:]), reads=["psA%d" % a], writes=[dn])
                    if j != 2:
                        P.op("act", lambda e, a=a, dst=dst, tsl=tsl: e.activation(out=sqb[a], in_=dst[:, tsl], func=AF.Square), reads=[dn], writes=["sq%d" % a])
                    if pend is not None:
                        post2(*pend)
                    pend = (a, j, st, dst, dn) if j != 2 else None
                if pend is not None:
                    post2(*pend)
                if _STOP == 1:
                    continue
                for p in range(3):
                    for b0 in range(0, 16, 8):
                        t = (p * 2 + b0 // 8) % 2
                        pt = psT[t]

                        def trv(e, p=p, b0=b0, pt=pt):
                            for i in range(8):
                                ins = e.transpose(pt[:, i, :], vT[:, tokslice(p, b0 + i)], identb[:])
                            return ins
                        P.op("pe", trv, reads=["vT", "identb"], writes=["psT%d" % t])
                        P.op("dve", lambda e, p=p, b0=b0, pt=pt: e.tensor_copy(out=vtok[:, p, b0:b0 + 8, :], in_=pt[:, :, :]),
                             reads=["psT%d" % t], writes=["vtok"])
                if _STOP == 2:
                    continue
                units = [(p, qb0) for p in range(3) for qb0 in range(0, 16, 2)]

                def hasprev(p, qb):
                    return (p == 0 and qb > 0) or (p == 1 and qb % 4 > 0)

                def st_qk(u, p, qb0):
                    bank, bn = Sb[u % 2]
                    Sv = bank[:].rearrange("p (b h t) -> p b h t", b=2, h=2)

                    def fn(e):
                        for bl in range(2):
                            qb = qb0 + bl
                            tq = tokslice(p, qb)
                            ks = tokslice(p, qb - 1) if hasprev(p, qb) else tq
                            e.matmul(Sv[:, bl, 0, :], lhsT=kT[:, ks], rhs=qT[:, tq], start=True, stop=True)
                            ins = e.matmul(Sv[:, bl, 1, :], lhsT=kT[:, tq], rhs=qT[:, tq], start=True, stop=True)
                        return ins
                    P.op("pe", fn, reads=["kT", "qT"], writes=[bn])
                    pi = u % 3
                    mi = 2 if p == 2 else (1 if not hasprev(p, qb0) else 0)
                    P.op("act", lambda e: e.activation(out=pT[pi], in_=bank[:], func=AF.Exp), reads=[bn], writes=["pT%d" % pi])
                    P.op("pool", lambda e: e.tensor_tensor(out=pT[pi], in0=pT[pi], in1=amask[:, mi, :], op=ALU.mult),
                         reads=["pT%d" % pi, "amask"], writes=["pT%d" % pi])

                def st_pv(u, p, qb0):
                    bank, bn = Ob[u % 2]
                    Ov = bank[:].rearrange("p (b o t) -> p b o t", b=2, o=2)
                    pi = u % 3

                    def fn(e):
                        for bl in range(2):
                            qb = qb0 + bl
                            hp = hasprev(p, qb)
                            for o, lh in ((0, None), (1, onesb[:])):
                                if hp:
                                    e.matmul(Ov[:, bl, o, :], lhsT=(vtok[:, p, qb - 1, :] if o == 0 else lh), rhs=pT[pi][:, bl * 256:bl * 256 + 128],
                                             start=True, stop=False)
                                ins = e.matmul(Ov[:, bl, o, :], lhsT=(vtok[:, p, qb, :] if o == 0 else lh), rhs=pT[pi][:, bl * 256 + 128:bl * 256 + 256],
                                               start=(not hp), stop=True)
                        return ins
                    P.op("pe", fn, reads=["vtok", "onesb", "pT%d" % pi], writes=[bn])
                    if p == 0:
                        dv = accB[:, :, qb0 * 128:qb0 * 128 + 256].rearrange("p o (b t) -> p b o t", b=2)
                    elif p == 1:
                        st0 = (qb0 // 4) + 512 * (qb0 % 4)
                        dv = accB[:, :, st0:st0 + 1021:4].rearrange("p o (b t) -> p b o t", b=2)
                    else:
                        dv = accB[:, :, :].rearrange("p o (i r) -> p r o i", r=16)[:, qb0:qb0 + 2, :, :]
                    if p == 0:
                        P.op("dve", lambda e: e.tensor_copy(out=dv, in_=Ov), reads=[bn], writes=["accB"])
                    else:
                        P.op("dve", lambda e: e.tensor_tensor(out=dv, in0=Ov, in1=dv, op=ALU.add), reads=[bn, "accB"], writes=["accB"])
                pend_u = []
                for u, (p, qb0) in enumerate(units):
                    st_qk(u, p, qb0)
                    pend_u.append((u, p, qb0))
                    if len(pend_u) > 2:
                        st_pv(*pend_u.pop(0))
                while pend_u:
                    st_pv(*pend_u.pop(0))
                for st in range(4):
                    a = st % 2
                    P.op("act", lambda e, a=a, st=st: e.activation(out=lnt[a], in_=accB[:, 1, st * 512:(st + 1) * 512], func=AF.Ln),
                         reads=["accB"], writes=["lnt%d" % a])
                    P.op("act", lambda e, a=a: e.activation(out=rs[a], in_=lnt[a], func=AF.Exp, scale=-1.0), reads=["lnt%d" % a], writes=["rs%d" % a])
                    P.op("dve", lambda e, a=a, st=st, h=h: e.tensor_tensor(out=oT[:, h, st * 512:(st + 1) * 512], in0=accB[:, 0, st * 512:(st + 1) * 512],
                                                                         in1=rs[a], op=ALU.mult),
                         reads=["accB", "rs%d" % a], writes=[("oT", h)])
            add_dbg("oT%d" % s, oT, [128, 8, S], [("oT", h) for h in range(8)])
            if _STOP:
                return R4B

            P.alias(R4C + ["sg0", "sg1", "tmpc0", "tmpc1"], R4B)
            P.alias([("mg", st) for st in range(4)], R2B)
            sg = [view(R4, 24576 + i * 2048, [128, 512], F32) for i in range(2)]
            gv = [view(R4, i * 4096, [128, 1024], F32) for i in range(4)]
            vn = view(R4, 16384, [128, 4, 1024], BF16)
            ug = sg

            for nm_ in ("Wgb", "Wpb", "Wvs"):
                if nm_ not in c4w:
                    c4w[nm_] = load16(*c4spec[nm_])
            Wgb, Wpb, Wvs = c4w["Wgb"], c4w["Wpb"], c4w["Wvs"]
            ci = 0
            for st in range(4):
                tsl = slice(st * 512, (st + 1) * 512)
                for ct in range(8):
                    a = ci % 2
                    ci += 1
                    csl = slice(ct * 128, (ct + 1) * 128)
                    mm(psA[a][:], [(Wgb[:, k, csl], hT[:, k, tsl]) for k in range(8)], reads=wres(0, 2) + [("hT", st)], writes=["psA%d" % a])
                    mm(psB[a][:], [(Wpb[:, k, csl], oT[:, k, tsl]) for k in range(8)], reads=wres(2, 2) + [("oT", k) for k in range(8)], writes=["psB%d" % a])
                    P.op("act", lambda e, a=a: e.activation(out=sg[a], in_=psA[a][:], func=AF.Sigmoid), reads=["psA%d" % a], writes=["sg%d" % a])
                    P.op("dve", lambda e, a=a, ct=ct, tsl=tsl: e.tensor_tensor(out=mg[:, ct, tsl], in0=psB[a][:], in1=sg[a], op=ALU.mult),
                         reads=["psB%d" % a, "sg%d" % a], writes=[("mg", st)])
            P.alias([("zT", st) for st in range(4)], [("oT", h) for h in range(8)])
            Wu = load16(0, w_in[:, 0:D])
            Wga = load16(2, w_in[:, 5 * D:6 * D])
            for st in range(4):
                tsl = slice(st * 512, (st + 1) * 512)
                P.op("dve", lambda e: e.memset(ssV[:], 0.0), writes=[("ssV", i) for i in range(4)])
                for t4 in range(4):
                    tt = st * 4 + t4
                    for half in range(2):
                        mm(psA[half][:], [(hT[:, k, tt * 128:(tt + 1) * 128], Wvs[:, k, half * 512:(half + 1) * 512]) for k in range(8)],
                           reads=wres(4, 2) + [("hT", st)], writes=["psA%d" % half])
                        P.op("act", lambda e, half=half, t4=t4: e.activation(out=gv[t4][:, half * 512:(half + 1) * 512], in_=psA[half][:], func=AF.Gelu_apprx_tanh),
                             reads=["psA%d" % half], writes=["gv%d" % t4])
                    P.op("act", lambda e, t4=t4: e.activation(out=junk[:], in_=gv[t4], func=AF.Square, accum_out=ssV[:, t4:t4 + 1]),
                         reads=["gv%d" % t4], writes=["junk", ("ssV", t4)])
                rstd_small(ssV[:], lnV[:], rsV[:], 1.0 / D, [("ssV", i) for i in range(4)], "rsV")
                for t4 in range(4):
                    P.op("dve", lambda e, t4=t4: e.scalar_tensor_tensor(out=vn[:, t4, :], in0=gv[t4], scalar=rsV[:, t4:t4 + 1], in1=gsgu[:], op0=ALU.mult, op1=ALU.mult),
                         reads=["gv%d" % t4, "rsV", "gsgu"], writes=["vn"])
                for g in range(8):
                    a = g % 2
                    csl = slice(g * 128, (g + 1) * 128)
                    mm(psB[a][:], [(Wu[:, k, csl], hT[:, k, tsl]) for k in range(8)], reads=wres(0, 2) + [("hT", st)], writes=["psB%d" % a])
                    P.op("act", lambda e, a=a: e.activation(out=ug[a], in_=psB[a][:], func=AF.Gelu_apprx_tanh), reads=["psB%d" % a], writes=["sg%d" % a])
                    pm = psS if a == 0 else psO
                    pmn = "psS" if a == 0 else "psO"
                    pmn2 = pmn

                    def mix(e, g=g, pm=pm, csl=csl):
                        for t4 in range(4):
                            e.matmul(pm[:, t4 * 128:(t4 + 1) * 128], lhsT=vn[:, t4, csl], rhs=WsT[:, g, :], start=True, stop=False)
                            ins = e.matmul(pm[:, t4 * 128:(t4 + 1) * 128], lhsT=onesb[0:33, :], rhs=bs2[0:33, csl], start=False, stop=True)
                        return ins
                    P.op("pe", mix, reads=["vn", "WsT", "onesb"] + BS2, writes=[pmn, pmn2])
                    P.op("dve", lambda e, a=a, g=g, pm=pm, tsl=tsl: e.tensor_tensor(out=zT[:, g, tsl], in0=pm[:], in1=ug[a], op=ALU.mult),
                         reads=[pmn, pmn2, "sg%d" % a], writes=[("zT", st)])
            add_dbg("zT%d" % s, zT, [128, 8, S], [("zT", st) for st in range(4)])
            Wpa = load16(4, w_pa)
            Wo = load16(0, w_out)
            tmpc = [view(R4, i * 2048, [128, 512], F32) for i in range(2)]
            P.alias(["tmpc0", "tmpc1"], ["gv0"])
            ci = 0
            for st in range(4):
                tsl = slice(st * 512, (st + 1) * 512)
                for ct in range(8):
                    a = ci % 2
                    ci += 1
                    csl = slice(ct * 128, (ct + 1) * 128)
                    mm(psA[a][:], [(Wga[:, k, csl], hT[:, k, tsl]) for k in range(8)], reads=wres(2, 2) + [("hT", st)], writes=["psA%d" % a])
                    mm(psB[a][:], [(Wpa[:, k, csl], zT[:, k, tsl]) for k in range(8)], reads=wres(4, 2) + [("zT", st)], writes=["psB%d" % a])
                    P.op("act", lambda e, a=a: e.activation(out=sg[a], in_=psA[a][:], func=AF.Sigmoid), reads=["psA%d" % a], writes=["sg%d" % a])
                    P.op("dve", lambda e, a=a: e.tensor_tensor(out=tmpc[a], in0=psB[a][:], in1=sg[a], op=ALU.mult),
                         reads=["psB%d" % a, "sg%d" % a], writes=["tmpc%d" % a])
                    P.op("pool", lambda e, a=a, ct=ct, tsl=tsl: e.tensor_tensor(out=mg[:, ct, tsl], in0=tmpc[a], in1=mg[:, ct, tsl], op=ALU.add),
                         reads=["tmpc%d" % a, ("mg", st)], writes=[("mg", st)])
            add_dbg("mg%d" % s, mg, [128, 8, S], [("mg", st) for st in range(4)])
            R4E = ["xs0", "xs1", "xs2", "xnb0", "xnb1"]
            P.alias(R4E + RTN, R4C + ["sg0", "sg1", "tmpc0", "tmpc1"])
            P.alias([("acc", tt, hh) for tt in range(16) for hh in range(2)], [("hT", st) for st in range(4)] + [("zT", st) for st in range(4)])
            P.op("dve", lambda e: e.memset(ssA[:], 0.0), writes=[("ssA", i) for i in range(16)])
            P.op("dve", lambda e: e.tensor_tensor(out=Wo, in0=Wo, in1=gaterow[:, s, 0, :].unsqueeze(1).to_broadcast([128, 8, 1024]), op=ALU.mult),
                 reads=wres(0, 2) + ["gaterow"], writes=wres(0, 2))
            for tt in range(16):
                sl = tt % 3
                P.dma("sp", "x%d" % sl, lambda e, sl=sl, tt=tt: e.dma_start(out=xs[sl], in_=x_d[s, tt * 128:(tt + 1) * 128, :]),
                      writes=["xs%d" % sl])
                for half in range(2):
                    hs = slice(half * 512, (half + 1) * 512)
                    mm(psA[half][:], [(mg[:, k, tt * 128:(tt + 1) * 128], Wo[:, k, hs]) for k in range(8)],
                       reads=wres(0, 2) + [("mg", tt // 4), ("mgt", tt)], writes=["psA%d" % half])
                    P.op("dve", lambda e, half=half, hs=hs, tt=tt, sl=sl: e.tensor_tensor(out=acc[:, tt, hs], in0=psA[half][:], in1=xs[sl][:, hs], op=ALU.add),
                         reads=["psA%d" % half, "xs%d" % sl], writes=[("acc", tt, half)])
                norm_transpose(acc[:, tt, :], [("acc", tt, 0), ("acc", tt, 1)], tt, 1, s, h2T, ("h2T", tt), extra_w=[("mgt", tt)], defer=True)

                def c5_stage2(tt):
                    transpose_evac(tt, 1, s, h2T, ("h2T", tt), [("mgt", tt)])
                if tt >= 1:
                    c5_stage2(tt - 1)
            c5_stage2(15)
            lgv = psS[:, 0:320].rearrange("p (t n) -> p t n", n=20)

            def lg(e):
                for tt in range(16):
                    for k in range(8):
                        ins = e.matmul(lgv[:, tt, :], lhsT=h2T[:, k, tt * 128:(tt + 1) * 128], rhs=Wr[:, k, :], start=(k == 0), stop=(k == 7))
                return ins
            P.op("pe", lg, reads=[("h2T", tt) for tt in range(16)] + ["Wr"], writes=["psS"])
            P.op("dve", lambda e: e.tensor_tensor(out=logits[:], in0=lgv, in1=brow[:].unsqueeze(1).to_broadcast([128, 16, 20]), op=ALU.add),
                 reads=["psS", "brow"], writes=["logits"])
            add_dbg("x1_%d" % s, acc, [128, 16, D], [("acc", tt, hh) for tt in range(16) for hh in range(2)])
            add_dbg("h2T%d" % s, h2T, [128, 8, S], [("h2T", tt) for tt in range(16)])
            add_dbg("logits%d" % s, logits[:], [128, 16, 20], ["logits"])
            Lg = logits[:, :, 0:4]
            Le = logits[:, :, 4:20].rearrange("p t (g e) -> p t g e", e=4)

            def bc3(ap):
                return ap.unsqueeze(2).to_broadcast([128, 16, 4])

            def dv(fn, r, w):
                P.op("dve", fn, reads=["rt_" + n if n in rt else n for n in r], writes=["rt_" + n if n in rt else n for n in w])
            R = rt
            dv(lambda e: e.tensor_reduce(out=R["gmax"], in_=Lg, axis=AX.X, op=ALU.max), ["logits"], ["gmax"])
            dv(lambda e: e.tensor_tensor(out=R["ohg"], in0=Lg, in1=bc3(R["gmax"]), op=ALU.is_equal), ["logits", "gmax"], ["ohg"])
            dv(lambda e: e.tensor_copy(out=R["m1"], in_=R["ohg"][:, :, 0]), ["ohg"], ["m1"])
            for g_ in range(1, 4):
                dv(lambda e: e.tensor_scalar(out=R["m2"], in0=R["m1"], scalar1=-1.0, scalar2=1.0, op0=ALU.mult, op1=ALU.add), ["m1"], ["m2"])
                dv(lambda e, g_=g_: e.tensor_tensor(out=R["ohg"][:, :, g_], in0=R["ohg"][:, :, g_], in1=R["m2"], op=ALU.mult), ["ohg", "m2"], ["ohg"])
                if g_ < 3:
                    dv(lambda e, g_=g_: e.tensor_tensor(out=R["m1"], in0=R["m1"], in1=R["ohg"][:, :, g_], op=ALU.max), ["m1", "ohg"], ["m1"])
            dv(lambda e: e.tensor_tensor(out=R["dg"], in0=Lg, in1=bc3(R["gmax"]), op=ALU.subtract), ["logits", "gmax"], ["dg"])
            P.op("act", lambda e: e.activation(out=R["eg"], in_=R["dg"], func=AF.Exp), reads=["rt_dg"], writes=["rt_eg"])
            dv(lambda e: e.tensor_reduce(out=R["sumg"], in_=R["eg"], axis=AX.X, op=ALU.add), ["eg"], ["sumg"])
            dv(lambda e: e.reciprocal(out=R["pg"], in_=R["sumg"]), ["sumg"], ["pg"])
            dv(lambda e: e.tensor_tensor(out=R["tmp"], in0=Le, in1=R["ohg"].unsqueeze(3).to_broadcast([128, 16, 4, 4]), op=ALU.mult), ["logits", "ohg"], ["tmp"])
            dv(lambda e: e.tensor_reduce(out=R["sel"], in_=R["tmp"].rearrange("p t g e -> p t e g"), axis=AX.X, op=ALU.add), ["tmp"], ["sel"])
            dv(lambda e: e.tensor_reduce(out=R["m1"], in_=R["sel"], axis=AX.X, op=ALU.max), ["sel"], ["m1"])
            dv(lambda e: e.tensor_tensor(out=R["oh1"], in0=R["sel"], in1=bc3(R["m1"]), op=ALU.is_equal), ["sel", "m1"], ["oh1"])
            dv(lambda e: e.scalar_tensor_tensor(out=R["sel2"], in0=R["oh1"], scalar=-1e30, in1=R["sel"], op0=ALU.mult, op1=ALU.add), ["oh1", "sel"], ["sel2"])
            dv(lambda e: e.tensor_reduce(out=R["m2"], in_=R["sel2"], axis=AX.X, op=ALU.max), ["sel2"], ["m2"])
            dv(lambda e: e.tensor_tensor(out=R["oh2"], in0=R["sel2"], in1=bc3(R["m2"]), op=ALU.is_equal), ["sel2", "m2"], ["oh2"])
            dv(lambda e: e.tensor_tensor(out=R["d"], in0=R["m2"], in1=R["m1"], op=ALU.subtract), ["m2", "m1"], ["d"])
            P.op("act", lambda e: e.activation(out=R["ed"], in_=R["d"], func=AF.Exp), reads=["rt_d"], writes=["rt_ed"])
            dv(lambda e: e.tensor_scalar(out=R["den"], in0=R["ed"], scalar1=1.0, scalar2=None, op0=ALU.add), ["ed"], ["den"])
            dv(lambda e: e.reciprocal(out=R["r"], in_=R["den"]), ["den"], ["r"])
            dv(lambda e: e.tensor_tensor(out=R["p1"], in0=R["r"], in1=R["pg"], op=ALU.mult), ["r", "pg"], ["p1"])
            dv(lambda e: e.tensor_tensor(out=R["p2"], in0=R["ed"], in1=R["p1"], op=ALU.mult), ["ed", "p1"], ["p2"])
            dv(lambda e: e.tensor_tensor(out=R["c1"], in0=R["oh1"], in1=bc3(R["p1"]), op=ALU.mult), ["oh1", "p1"], ["c1"])
            dv(lambda e: e.tensor_tensor(out=R["c2"], in0=R["oh2"], in1=bc3(R["p2"]), op=ALU.mult), ["oh2", "p2"], ["c2"])
            dv(lambda e: e.tensor_tensor(out=R["cwg"], in0=R["c1"], in1=R["c2"], op=ALU.add), ["c1", "c2"], ["cwg"])
            cw4 = cw[:, :, 0:16].rearrange("p t (g e) -> p t g e", e=4)
            dv(lambda e: e.tensor_tensor(out=cw4, in0=R["ohg"].unsqueeze(3).to_broadcast([128, 16, 4, 4]),
                                         in1=R["cwg"].unsqueeze(2).to_broadcast([128, 16, 4, 4]), op=ALU.mult), ["ohg", "cwg"], ["cw"])
            dv(lambda e: e.tensor_copy(out=cw[:, :, 16], in_=rsA[:, 0:16]), ["rsA%d" % i for i in range(16)] + ["cw"], ["cw"])
            add_dbg("cw%d" % s, cw[:, :, 0:16], [128, 16, 16], ["cw"])

            dv(lambda e: e.tensor_copy(out=ohb[:], in_=R["ohg"]), ["ohg"], ["ohb"])
            cntv = psS[:, 0:64].rearrange("p (t g) -> p t g", g=4)

            def cums(e):
                for i in range(16):
                    for j in range(i):
                        e.matmul(cntv[:, i, :], lhsT=onesb[:], rhs=ohb[:, j, :], start=(j == 0), stop=False)
                    ins = e.matmul(cntv[:, i, :], lhsT=utri[:], rhs=ohb[:, i, :], start=(i == 0), stop=True)
                return ins
            P.op("pe", cums, reads=["ohb", "onesb", "utri"], writes=["psS"])
            mm(psO[:, 0:4], [(onesb[:], ohb[:, j, :]) for j in range(16)], reads=["ohb", "onesb"], writes=["psO"])
            dv(lambda e: e.tensor_copy(out=tot[:], in_=psO[:, 0:4]), ["psO"], ["tot"])
            dv(lambda e: e.tensor_copy(out=cnt3[:], in_=cntv), ["psS"], ["cnt3"])
            dv(lambda e: e.memset(stt[:], -1.0), [], ["stt"])
            for g in range(1, 4):
                dv(lambda e, g=g: e.tensor_tensor(out=stt[:, g:g + 1], in0=stt[:, g - 1:g], in1=tot[:, g - 1:g], op=ALU.add), ["stt", "tot"], ["stt"])
            dv(lambda e: e.tensor_tensor(out=ent[:], in0=stt[:], in1=tot[:], op=ALU.add), ["stt", "tot"], ["ent"])
            dv(lambda e: e.tensor_tensor(out=cnt3[:], in0=cnt3[:], in1=stt[:].unsqueeze(1).to_broadcast([128, 16, 4]), op=ALU.add), ["cnt3", "stt"], ["cnt3"])
            dv(lambda e: e.tensor_tensor(out=cnt3[:], in0=cnt3[:], in1=R["ohg"], op=ALU.mult), ["cnt3", "ohg"], ["cnt3"])
            dv(lambda e: e.tensor_reduce(out=posf[:], in_=cnt3[:], axis=AX.X, op=ALU.add), ["cnt3"], ["posf"])
            dv(lambda e: e.tensor_copy(out=posi[:], in_=posf[:]), ["posf"], ["posi"])
            dv(lambda e: e.tensor_tensor(out=flA[:], in0=stt[:].unsqueeze(2).to_broadcast([128, 4, 8]),
                                         in1=blkc[:, 0, :].unsqueeze(1).to_broadcast([128, 4, 8]), op=ALU.is_lt), ["stt", "blkc"], ["flA"])
            dv(lambda e: e.tensor_tensor(out=flB[:], in0=ent[:].unsqueeze(2).to_broadcast([128, 4, 8]),
                                         in1=blkc[:, 1, :].unsqueeze(1).to_broadcast([128, 4, 8]), op=ALU.is_ge), ["ent", "blkc"], ["flB"])
            dv(lambda e: e.tensor_tensor(out=flA[:], in0=flA[:], in1=flB[:], op=ALU.mult), ["flA", "flB"], ["flA"])
            dv(lambda e: e.tensor_copy(out=flags_i[:], in_=flA[:].rearrange("p g b -> p (g b)")), ["flA"], ["flags"])
            dv(lambda e: e.tensor_reduce(out=flQ[:], in_=flA[:].rearrange("p g (q b) -> p g q b", b=4), axis=AX.X, op=ALU.max), ["flA"], ["flQ"])
            dv(lambda e: e.tensor_copy(out=flags_q[:], in_=flQ[:].rearrange("p g q -> p (g q)")), ["flQ"], ["flags"])
            add_dbg("posf%d" % s, posf[:], [128, 16], ["posf"])
            add_dbg("flA%d" % s, flA[:], [128, 4, 8], ["flA"])
            for tt in range(16):
                idx = bass.IndirectOffsetOnAxis(ap=posi[:, tt:tt + 1], axis=0)
                P.dma("pool", "sc1", lambda e, tt=tt, idx=idx: e.indirect_dma_start(out=X1_d[s], out_offset=idx, in_=acc[:, tt, :], in_offset=None),
                      reads=[("acc", tt, 0), ("acc", tt, 1), "posi"], writes=["X1"])
                P.dma("pool", "scc", lambda e, tt=tt, idx=idx: e.indirect_dma_start(out=CW_d[s], out_offset=idx, in_=cw[:, tt, :], in_offset=None),
                      reads=["cw", "posi"], writes=["CW"])
            P.dma("sp", "rbc", lambda e: e.dma_start(out=cw[:], in_=CW_d[s].rearrange("(t p) e -> p t e", p=128)), reads=["CW"], writes=["cw"])
            for j in range(16):
                P.dma("sp", "rb%d" % j, lambda e, j=j: e.dma_start(out=acc[:, j, :], in_=X1_d[s][j * 128:(j + 1) * 128, :]),
                      reads=["X1"], writes=[("acc", j, 0), ("acc", j, 1)])
            for j in range(16):
                P.op("dve", lambda e, j=j: e.tensor_scalar(out=xnb[j % 2], in0=acc[:, j, :], scalar1=cw[:, j, 16:17], scalar2=None, op0=ALU.mult),
                     reads=[("acc", j, 0), ("acc", j, 1), "cw"], writes=["xnb%d" % (j % 2)])
                transpose_evac(j, 1, s, h2T, ("h2T", j))
            P.alias(R4D, R4E)
            heT = [view(R4, i * 4096, [128, 4, 512], BF16) for i in range(2)]
            sgD = [view(R4, 8192 + i * 2048, [128, 512], F32) for i in range(2)]
            psY = [psS, psO]
            psYn = [["psS"], ["psO"]]

            stg = [view(R4, 12288 + i * 8192, [128, 2048], F32) for i in range(2)]
            stgc = [0]

            def load_expert(e_):
                b = (3 * e_) % 6
                Wg = wslot(b, [128, 8, 512]); Wu_ = wslot((b + 1) % 6, [128, 8, 512]); Wd = wslot((b + 2) % 6, [128, 4, 1024])
                dmas, casts = [], []
                for mi, (Wdst, src) in enumerate(((Wg, w_gate), (Wu_, w_up), (Wd, w_down))):
                    for hh in range(2):
                        si = stgc[0] % 2
                        stgc[0] += 1
                        sres = "stg%d" % si
                        if mi < 2:
                            sv = stg[si].rearrange("p (k n) -> p k n", n=512)
                            srcap = src[e_, hh * 512:(hh + 1) * 512, :].rearrange("(k p) n -> p k n", p=128)
                            dstap = Wdst[:, hh * 4:(hh + 1) * 4, :]
                        else:
                            sv = stg[si].rearrange("p (k n) -> p k n", n=1024)
                            srcap = src[e_, hh * 256:(hh + 1) * 256, :].rearrange("(k p) n -> p k n", p=128)
                            dstap = Wdst[:, hh * 2:(hh + 1) * 2, :]
                        dmas.append(lambda sres=sres, sv=sv, srcap=srcap: P.dma(
                            "sp", sres, lambda e: e.dma_start(out=sv, in_=srcap), writes=[sres]))
                        if mi < 2:
                            casts.append(lambda sres=sres, sv=sv, dstap=dstap, mi=mi: P.op(
                                "pool", lambda e: e.tensor_tensor(out=dstap, in0=sv, in1=ones1[:, 0:1].unsqueeze(2).to_broadcast([128, 4, 512]), op=ALU.mult),
                                reads=[sres, "ones1"], writes=wres(b + mi)))
                        else:
                            casts.append(lambda sres=sres, sv=sv, dstap=dstap, mi=mi: P.op("pool", lambda e: e.tensor_tensor(
                                out=dstap, in0=sv, in1=gaterow[:, s, 1, :].unsqueeze(1).to_broadcast([128, 2, 1024]), op=ALU.mult),
                                reads=[sres, "gaterow"], writes=wres(b + mi)))
                d, c = dmas, casts
                steps = [d[0], d[1], c[0], d[2], c[1], d[3], c[2], d[4], c[3], d[5], c[4], c[5]]
                return (Wg, Wu_, Wd, b), steps
            nxt, steps0 = load_expert(0)
            for st_ in steps0:
                st_()
            ci = 0
            yi = 0
            hbi = 0
            for e_ in range(N_EXP):
                Wg, Wu_, Wd, b = nxt
                rest = []
                if e_ + 1 < N_EXP:
                    nxt, rest = load_expert(e_ + 1)
                    rest = list(rest)
                    while rest:
                        rest.pop(0)()
                sched = [2, 1, 1, 1, 1, 1, 1, 2]
                g = e_ // 4
                for blk in range(4):
                    P.cond_begin(flags_i[0:1, g * 8 + blk:g * 8 + blk + 1], "flags")
                    tsl = slice(blk * 512, (blk + 1) * 512)
                    hb = hbi % 2
                    hbi += 1
                    h2r = [("h2T", blk * 4 + i) for i in range(4)]
                    for f in range(4):
                        a = ci % 2
                        ci += 1
                        fsl = slice(f * 128, (f + 1) * 128)
                        mm(psA[a][:], [(Wg[:, k, fsl], h2T[:, k, tsl]) for k in range(8)], reads=wres(b) + h2r, writes=["psA%d" % a])
                        mm(psB[a][:], [(Wu_[:, k, fsl], h2T[:, k, tsl]) for k in range(8)], reads=wres(b + 1) + h2r, writes=["psB%d" % a])
                        P.op("act", lambda e, a=a: e.activation(out=sgD[a], in_=psA[a][:], func=AF.Silu), reads=["psA%d" % a], writes=["sgD%d" % a])
                        P.op("dve", lambda e, a=a, hb=hb, f=f: e.tensor_tensor(out=heT[hb][:, f, :], in0=psB[a][:], in1=sgD[a], op=ALU.mult),
                             reads=["psB%d" % a, "sgD%d" % a], writes=["heT%d" % hb])
                    for t4 in range(4):
                        tt = blk * 4 + t4
                        for half in range(2):
                            y = yi % 2
                            yi += 1
                            hs = slice(half * 512, (half + 1) * 512)
                            mm(psY[y][:], [(heT[hb][:, f, t4 * 128:(t4 + 1) * 128], Wd[:, f, hs]) for f in range(4)],
                               reads=wres(b + 2) + ["heT%d" % hb], writes=psYn[y])
                            P.op("dve", lambda e, y=y, tt=tt, hs=hs, e_=e_: e.scalar_tensor_tensor(
                                out=acc[:, tt, hs], in0=psY[y][:], scalar=cw[:, tt, e_:e_ + 1], in1=acc[:, tt, hs], op0=ALU.mult, op1=ALU.add),
                                reads=psYn[y] + ["cw", ("acc", tt, half)], writes=[("acc", tt, half)])
                    P.cond_end()
            ot = [view(R4, 16384 + i * 4096, [128, 1024], F32) for i in range(3)]
            OTN = ["ot0", "ot1", "ot2"]
            P.alias(OTN, R4D + R4E + RTN)
            for j in range(16):
                P.dma("sp", "sy", lambda e, j=j: e.dma_start(out=Y_d[s][j * 128:(j + 1) * 128, :], in_=acc[:, j, :]), reads=[("acc", j, 0), ("acc", j, 1)], writes=["Y"])
            for tt in range(16):
                o = tt % 3
                idx = bass.IndirectOffsetOnAxis(ap=posi[:, tt:tt + 1], axis=0)
                P.dma("pool", "gy%d" % o, lambda e, o=o, idx=idx: e.indirect_dma_start(out=ot[o], out_offset=None, in_=Y_d[s], in_offset=idx),
                      reads=["Y", "posi"], writes=[OTN[o]])
                P.dma("sp", "st%d" % tt, lambda e, tt=tt, o=o: e.dma_start(out=y_d[s, tt * 128:(tt + 1) * 128, :], in_=ot[o]),
                      reads=[OTN[o]], final=True)
            return R4D + R4E
        prevR4 = R4P
        for s_ in range(nseq):
            prevR4 = seq_body(s_, prevR4)
        P.emit()
    return nc


_CACHE = {}


def _consts():
    tril = np.tril(np.ones((128, 128), np.float32))
    trilT = np.ascontiguousarray(tril.T)
    tk = np.arange(128)[:, None]
    tq = np.arange(128)[None, :]
    pm = (tk >= tq).astype(np.float32); cm = (tk <= tq).astype(np.float32); z = np.zeros_like(pm)
    amask = np.stack([np.concatenate([pm, cm, pm, cm], 1), np.concatenate([z, cm, pm, cm], 1), np.concatenate([z, cm, z, cm], 1)], 1)
    amask = np.ascontiguousarray(amask).astype(ml_dtypes.bfloat16)
    blk = np.arange(8, dtype=np.float32) * 512
    blkc = np.ascontiguousarray(np.broadcast_to(np.stack([blk + 511, blk], 0)[None], (128, 2, 8))).astype(np.float32)
    return {
        "utri": np.triu(np.ones((128, 128), np.float32)).astype(ml_dtypes.bfloat16),
        "blkc": blkc,
        "identb": np.eye(128, dtype=np.float32).astype(ml_dtypes.bfloat16),
        "identf": np.eye(128, dtype=np.float32),
        "trilT": trilT,
        "amask": amask,
    }


def make_in_maps(inp, n_cores=8):
    f = lambda a: np.ascontiguousarray(np.asarray(a, dtype=np.float32))
    w_r = np.concatenate([f(inp["w_router_group"])[0]] + [f(inp["w_router_expert"])[0, g] for g in range(4)], axis=1)
    b_r = np.concatenate([f(inp["b_router_group"])[0].reshape(-1), f(inp["b_router_expert"])[0].reshape(-1)])
    shared = {
        "w_ada": f(inp["w_ada"])[0], "b_ada": f(inp["b_ada"])[0], "norm1_g": f(inp["norm1_g"])[0],
        "w_in": f(inp["w_in"])[0], "sgu_norm_g": f(inp["sgu_norm_g"])[0],
        "sgu_wT": np.ascontiguousarray(f(inp["sgu_w"])[0].transpose(0, 2, 1)),
        "sgu_b": f(inp["sgu_b"])[0].reshape(-1), "q_norm_g": f(inp["q_norm_g"])[0], "k_norm_g": f(inp["k_norm_g"])[0],
        "w_proj_a": f(inp["w_proj_a"])[0], "w_proj_b": f(inp["w_proj_b"])[0], "w_out": f(inp["w_out"])[0],
        "norm2_g": f(inp["norm2_g"])[0], "w_r": np.ascontiguousarray(w_r), "b_r": np.ascontiguousarray(b_r),
        "w_gate": f(inp["w_gate"])[0], "w_up": f(inp["w_up"])[0], "w_down": f(inp["w_down"])[0],
    }
    shared.update(_consts())
    x = f(inp["x"]); c = f(inp["c"])
    maps = []
    for i in range(n_cores):
        m = dict(shared)
        m["x"] = np.ascontiguousarray(x[NSEQ * i:NSEQ * (i + 1)])
        m["c"] = np.ascontiguousarray(c[NSEQ * i:NSEQ * (i + 1)])
        maps.append(m)
    return maps


def kernel(**inputs):
    if "nc" not in _CACHE:
        _CACHE["nc"] = build()
    nc = _CACHE["nc"]
    maps = make_in_maps(inputs)
    res = run_bass_kernel_spmd(nc, maps, core_ids=list(range(8)))
    return np.concatenate([r["y"] for r in res.results], axis=0).astype(np.float32)
```

```python
import contextlib
import numpy as np
import ml_dtypes
import concourse.bass as bass
import concourse.mybir as mybir
from concourse.bass_utils import run_bass_kernel_spmd

F32 = mybir.dt.float32
BF16 = mybir.dt.bfloat16
AF = mybir.ActivationFunctionType
ALU = mybir.AluOpType
AX = mybir.AxisListType

NSEQ = 2
_STOP = 0
S = 2048
D = 1024
EPS = 1e-6
N_EXP = 16


class Prog:
    ENGS = ("pe", "act", "dve", "pool", "sp")

    def __init__(self, nc):
        self.nc = nc
        self.ops = {e: [] for e in self.ENGS}
        self.cnt = {}
        self.seen = {e: {} for e in self.ENGS}
        self.res = {}
        self.sems = {}
        self.final = []
        self.cond_stack = []
        self.cond_first = {}
        self.ncond = 0

    def cond_begin(self, flag_ap, flag_res):
        self.ncond += 1
        self.cond_stack.append((self.ncond, flag_ap, flag_res))

    def cond_end(self):
        self.cond_stack.pop()

    def _deps(self, eng, reads, writes):
        deps = {}

        def addall(d):
            for s, v in d.items():
                if deps.get(s, 0) < v:
                    deps[s] = v
        for r in reads:
            st = self.res.get(r)
            if st is not None:
                addall(st[0])
        for w in writes:
            st = self.res.get(w)
            if st is not None:
                addall(st[0])
                addall(st[1])
        waits = []
        for s, v in deps.items():
            if s == eng and eng == "pe":
                continue
            if self.seen[eng].get(s, 0) >= v:
                continue
            self.seen[eng][s] = v
            waits.append((s, v))
        return waits

    def _commit(self, ev, reads, writes):
        s, v = ev
        for r in reads:
            st = self.res.setdefault(r, [{}, {}])
            st[1][s] = max(st[1].get(s, 0), v)
        for w in writes:
            st = self.res.setdefault(w, [{}, {}])
            st[0][s] = max(st[0].get(s, 0), v)

    def alias(self, new, old):
        ev = {}
        for o in old:
            st = self.res.get(o)
            if st is None:
                continue
            for d in st:
                for s, v in d.items():
                    ev[s] = max(ev.get(s, 0), v)
        for n in new:
            st = self.res.setdefault(n, [{}, {}])
            for s, v in ev.items():
                st[0][s] = max(st[0].get(s, 0), v)

    def op(self, eng, fn, reads=(), writes=()):
        cond = None
        if self.cond_stack:
            for (cid, flag_ap, flag_res) in self.cond_stack:
                if (cid, eng) not in self.cond_first:
                    self.cond_first[(cid, eng)] = self._deps(eng, [flag_res], [])
            saved = dict(self.seen[eng])
            waits = self._deps(eng, reads, writes)
            self.seen[eng] = saved
            cond = tuple((cid, flag_ap) for (cid, flag_ap, _) in self.cond_stack)
        else:
            waits = self._deps(eng, reads, writes)
        self.cnt[eng] = self.cnt.get(eng, 0) + 1
        ev = (eng, self.cnt[eng])
        self.ops[eng].append((waits, fn, eng, 1, cond, self.cnt[eng]))
        self._commit(ev, reads, writes)
        return ev

    def dma(self, q, sem, fn, reads=(), writes=(), final=False):
        waits = self._deps(q, reads, writes)
        self.cnt[sem] = self.cnt.get(sem, 0) + 16
        ev = (sem, self.cnt[sem])
        self.ops[q].append((waits, fn, sem, 16, None, self.cnt[sem]))
        self._commit(ev, reads, writes)
        if final:
            self.final.append(ev)
        return ev

    def emit(self):
        nc = self.nc
        keys = list(self.cnt.keys())
        with contextlib.ExitStack() as es:
            for k in keys:
                self.sems[k] = es.enter_context(nc.semaphore("s_" + str(k)))
            block = es.enter_context(nc.Block())
            sems = self.sems

            regs = {"pe": [es.enter_context(nc.tensor.register("rg_pe%d" % i)) for i in range(2)],
                    "act": [es.enter_context(nc.scalar.register("rg_act%d" % i)) for i in range(2)],
                    "dve": [es.enter_context(nc.vector.register("rg_dve%d" % i)) for i in range(2)]}

            def emit_one(e, item):
                waits, fn, semk, inc, _, _c = item
                for s, v in waits:
                    e.wait_ge(sems[s], v)
                fn(e).then_inc(sems[semk], inc)

            def emit_range(e, engname, ops, i, j, depth):
                k = i
                while k < j:
                    chain = ops[k][4] or ()
                    if len(chain) <= depth:
                        emit_one(e, ops[k])
                        k += 1
                        continue
                    cid, ap = chain[depth]
                    m = k
                    while m < j and ops[m][4] is not None and len(ops[m][4]) > depth and ops[m][4][depth][0] == cid:
                        m += 1
                    for s, v in self.cond_first[(cid, engname)]:
                        e.wait_ge(sems[s], v)
                    r = regs[engname][depth]
                    e.reg_load(r, ap)
                    with e.If_ne(r, 0):
                        emit_range(e, engname, ops, k, m, depth + 1)
                    with e.Else():
                        e.wait_ge(sems[engname], ops[k][5] - 1)
                        e.sem_inc(sems[engname], m - k)
                    k = m

            def replay(engname, e):
                ops = self.ops[engname]
                emit_range(e, engname, ops, 0, len(ops), 0)
                if engname == "sp":
                    fin = {}
                    for s, v in self.final:
                        fin[s] = max(fin.get(s, 0), v)
                    for s, v in fin.items():
                        e.wait_ge(sems[s], v)

            @block.sync
            def _(e):
                replay("sp", e)

            @block.tensor
            def _(e):
                replay("pe", e)

            @block.scalar
            def _(e):
                replay("act", e)

            @block.vector
            def _(e):
                replay("dve", e)

            @block.gpsimd
            def _(e):
                replay("pool", e)


def tokslice(p, b):
    if p == 0:
        return slice(b * 128, (b + 1) * 128)
    if p == 1:
        r, blk = b // 4, b % 4
        st = r + 512 * blk
        return slice(st, st + 4 * 127 + 1, 4)
    return slice(b, b + 16 * 127 + 1, 16)


def build(nseq=NSEQ, dbg=()):
    nc = bass.Bass("TRN2", target_bir_lowering=False)
    P = Prog(nc)

    def din(name, shape, dt=F32):
        return nc.dram_tensor(name, shape, dt, kind="ExternalInput").ap()
    x_d = din("x", [NSEQ, S, D]); c_d = din("c", [NSEQ, D])
    w_ada = din("w_ada", [D, 6 * D]); b_ada = din("b_ada", [6 * D]); n1g = din("norm1_g", [D])
    w_in = din("w_in", [D, 7 * D]); sgu_g = din("sgu_norm_g", [D]); sgu_wT = din("sgu_wT", [8, 128, 128])
    sgu_b = din("sgu_b", [8 * 128]); qg = din("q_norm_g", [128]); kg = din("k_norm_g", [128])
    w_pa = din("w_proj_a", [D, D]); w_pb = din("w_proj_b", [D, D]); w_out = din("w_out", [D, D])
    n2g = din("norm2_g", [D]); w_r = din("w_r", [D, 20]); b_r = din("b_r", [20])
    w_gate = din("w_gate", [N_EXP, D, 512]); w_up = din("w_up", [N_EXP, D, 512]); w_down = din("w_down", [N_EXP, 512, D])
    identb_d = din("identb", [128, 128], BF16); identf_d = din("identf", [128, 128])
    trilT_d = din("trilT", [128, 128]); amask_d = din("amask", [128, 3, 512], BF16)
    utri_d = din("utri", [128, 128], BF16); blkc_d = din("blkc", [128, 2, 8])
    y_d = nc.dram_tensor("y", [NSEQ, S, D], F32, kind="ExternalOutput").ap()
    X1_d = [nc.dram_tensor("scr_x1%d" % i, [S, D], F32, kind="Internal").ap() for i in range(NSEQ)]
    CW_d = [nc.dram_tensor("scr_cw%d" % i, [S, 17], F32, kind="Internal").ap() for i in range(NSEQ)]
    Y_d = [nc.dram_tensor("scr_y%d" % i, [S, D], F32, kind="Internal").ap() for i in range(NSEQ)]
    dbg_out = {}

    with contextlib.ExitStack() as es:
        def sb(name, shape, dt=F32):
            return es.enter_context(nc.sbuf_tensor("sb_" + name, shape, dt))

        def ps(name, shape, dt=F32):
            return es.enter_context(nc.psum_tensor("ps_" + name, shape, dt))
        R01 = sb("R01", [128, 32768], BF16)
        R2 = sb("R2", [128, 16384], BF16)
        R4 = sb("R4", [128, 14336], BF16)
        Wt = sb("Wt", [128, 6 * 4096], BF16)
        identb = sb("identb", [128, 128], BF16); identf = sb("identf", [128, 128])
        onesb = sb("onesb", [128, 128], BF16); amask = sb("amask", [128, 3, 512], BF16)
        vecs = sb("vecs", [80, 128]); colsT = sb("colsT", [128, 80]); condb = sb("condb", [128, 16], BF16)
        modc = sb("modc", [128, 48, 2]); Gc = sb("Gc", [128, 2, 2, 8])
        gaterow = sb("gaterow", [128, 2, 2, 1024])
        gsgu = sb("gsgu", [128, 1024]); WsT = sb("WsT", [128, 8, 128], BF16)
        bs2 = sb("bs2", [33, 1024], BF16); gqk = sb("gqk", [128, 2]); epsT = sb("epsT", [128, 1]); ones1 = sb("ones1", [128, 1])
        Wr = sb("Wr", [128, 8, 20], BF16); brow = sb("brow", [128, 20])
        logits = sb("logits", [128, 16, 20]); cw = sb("cw", [128, 16, 17])
        junk = sb("junk", [128, 1024], BF16)
        I32 = mybir.dt.int32
        utri = sb("utri", [128, 128], BF16); blkc = sb("blkc", [128, 2, 8]); ohb = sb("ohb", [128, 16, 4], BF16)
        tot = sb("tot", [128, 4]); stt = sb("stt", [128, 4]); ent = sb("ent", [128, 4]); cnt3 = sb("cnt3", [128, 16, 4])
        posf = sb("posf", [128, 16]); posi = sb("posi", [128, 16], I32)
        flA = sb("flA", [128, 4, 8]); flB = sb("flB", [128, 4, 8]); flags_i = sb("flags_i", [128, 32], I32)
        flQ = sb("flQ", [128, 4, 2]); flags_q = sb("flags_q", [128, 8], I32)
        ssA = sb("ssA", [128, 16]); lnA = sb("lnA", [128, 16]); rsA = sb("rsA", [128, 16])
        ssV = sb("ssV", [128, 4]); lnV = sb("lnV", [128, 4]); rsV = sb("rsV", [128, 4])
        psA = [ps("psA0", [128, 512]), ps("psA1", [128, 512])]
        psB = [ps("psB0", [128, 512]), ps("psB1", [128, 512])]
        psT = [ps("psT0", [128, 8, 128], BF16), ps("psT1", [128, 8, 128], BF16)]
        psS = ps("psS", [128, 512]); psO = ps("psO", [128, 512])

        def view(reg, off, shape, dt):
            n = int(np.prod(shape[1:]))
            nb = n * (4 if dt == F32 else 2)
            ap = reg[:, off // 2: (off + nb) // 2]
            if dt == F32:
                ap = ap.bitcast(F32)
            if len(shape) == 3:
                ap = ap.rearrange("p (a b) -> p a b", b=shape[2])
            elif len(shape) == 4:
                ap = ap.rearrange("p (a b c) -> p a b c", b=shape[2], c=shape[3])
            return ap
        rt = {}
        _o = 16384
        for n, sh in [("gmax", []), ("ohg", [4]), ("dg", [4]), ("eg", [4]), ("sumg", []), ("pg", []), ("tmp", [4, 4]),
                      ("sel", [4]), ("m1", []), ("oh1", [4]), ("sel2", [4]), ("m2", []), ("oh2", [4]), ("d", []),
                      ("ed", []), ("den", []), ("r", []), ("p1", []), ("p2", []), ("c1", [4]), ("c2", [4]), ("cwg", [4])]:
            shape = [128, 16] + sh
            rt[n] = view(R4, _o, shape, F32)
            _o += int(np.prod(shape[1:])) * 4
        RTN = ["rt_" + n for n in rt]
        K32 = 32768
        hT = view(R01, 0, [128, 8, S], BF16)
        oT = view(R01, K32, [128, 8, S], BF16)
        zT = oT
        acc = view(R01, 0, [128, 16, D], F32)
        mg = view(R2, 0, [128, 8, S], BF16)
        h2T = mg

        def wslot(i, shape):
            return view(Wt, i * 8192, shape, BF16)

        def wres(i, n=1):
            return [("W", (i + j) % 6) for j in range(n)]

        def mm(out, pairs, reads, writes):
            def fn(e, out=out, pairs=pairs):
                n = len(pairs)
                for i, (l, r) in enumerate(pairs):
                    ins = e.matmul(out, lhsT=l, rhs=r, start=(i == 0), stop=(i == n - 1))
                return ins
            P.op("pe", fn, reads, writes)

        def add_dbg(name, ap, shape, reads):
            if name in dbg:
                d = nc.dram_tensor("dbg_" + name, shape, ap.dtype, kind="ExternalOutput").ap()
                P.dma("sp", "dbg_" + name, lambda e, d=d, ap=ap: e.dma_start(out=d, in_=ap), reads=reads, final=True)

        cl = []

        def cdma(out, in_, res, q="sp"):
            P.dma(q, "const" if q == "sp" else "constp", lambda e, out=out, in_=in_: e.dma_start(out=out, in_=in_), writes=[res])
            if q == "sp":
                cl.append(res)
        cdma(identb[:], identb_d, "identb"); cdma(identf[:], identf_d, "identf"); cdma(amask[:], amask_d, "amask")
        cdma(vecs[0:8, :], n1g.rearrange("(k p) -> k p", p=128), "vecs")
        cdma(vecs[8:16, :], n2g.rearrange("(k p) -> k p", p=128), "vecs")
        cdma(vecs[16:64, :], b_ada.rearrange("(k p) -> k p", p=128), "vecs")
        cdma(vecs[64:80, :], c_d.rearrange("s (k p) -> (s k) p", p=128), "vecs")
        cdma(gqk[:, 0:1], qg.rearrange("(p o) -> p o", o=1), "gqk")
        cdma(gqk[:, 1:2], kg.rearrange("(p o) -> p o", o=1), "gqk")
        cdma(gsgu[:], sgu_g.partition_broadcast(128), "gsgu")
        cdma(brow[:], b_r.partition_broadcast(128), "brow")
        gbias = view(R2, 0, [128, 2, 1024], F32)
        cdma(gbias[:, 0, :], b_ada[2 * D:3 * D].partition_broadcast(128), "gbias")
        cdma(gbias[:, 1, :], b_ada[5 * D:6 * D].partition_broadcast(128), "gbias")
        wsf = view(R4, 8192, [128, 8, 128], F32)
        trilT = view(R4, 12288, [128, 128], F32)
        bsf = view(R4, 12800, [128, 1024], F32)
        bsh = view(R4, 16896, [128, 1024], F32)
        rep = view(R4, 20992, [128, 2, 8, 128], BF16)
        cdma(wsf, sgu_wT.rearrange("g s t -> s g t"), "wsf")
        cdma(trilT, trilT_d, "trilT")
        cdma(bsf[0:1, :], sgu_b.rearrange("(o n) -> o n", o=1), "bsf")
        cdma(bsf[32:33, :], sgu_b.rearrange("(o n) -> o n", o=1), "bsf")
        cdma(Wr[:], w_r.rearrange("(k p) n -> p k n", p=128), "Wr", q="pool")
        cdma(utri[:], utri_d, "utri"); cdma(blkc[:], blkc_d, "blkc")
        for r in set(cl):
            P.res[r][0]["const"] = P.cnt["const"]

        P.op("dve", lambda e: e.memset(onesb[:], 1.0), writes=["onesb"])
        P.op("dve", lambda e: e.memset(epsT[:], EPS), writes=["epsT"])
        P.op("dve", lambda e: e.memset(ones1[:], 1.0), writes=["ones1"])
        P.op("dve", lambda e: e.memset(bs2[:], 0.0), writes=["bs2"])
        P.op("dve", lambda e: e.tensor_tensor(out=WsT[:], in0=wsf, in1=trilT.unsqueeze(1).to_broadcast([128, 8, 128]), op=ALU.mult),
             reads=["wsf", "trilT"], writes=["WsT"])
        P.op("dve", lambda e: e.tensor_copy(out=bs2[0:1, :], in_=bsf[0:1, :]), reads=["bsf", "bs2"], writes=["bs2a"])
        P.op("dve", lambda e: e.tensor_copy(out=junk[32:33, :], in_=bsf[32:33, :]), reads=["bsf"], writes=["junk"])
        P.op("dve", lambda e: e.tensor_copy(out=bsh[32:33, :], in_=junk[32:33, :]), reads=["junk"], writes=["bsh"])
        P.op("dve", lambda e: e.tensor_tensor(out=bsh[32:33, :], in0=bsf[32:33, :], in1=bsh[32:33, :], op=ALU.subtract),
             reads=["bsh", "bsf"], writes=["bsh"])
        P.op("dve", lambda e: e.tensor_copy(out=bs2[32:33, :], in_=bsh[32:33, :]), reads=["bsh", "bs2"], writes=["bs2b"])
        BS2 = ["bs2a", "bs2b"]
        P.op("dve", lambda e: e.tensor_scalar(out=gqk[:, 0:1], in0=gqk[:, 0:1], scalar1=float(128 ** -0.5), scalar2=None, op0=ALU.mult),
             reads=["gqk"], writes=["gqk"])
        P.op("pe", lambda e: e.transpose(psA[0][:, 0:80], vecs[0:80, :], identf[0:80, 0:80]), reads=["vecs", "identf"], writes=["psA0"])
        P.op("dve", lambda e: e.tensor_copy(out=colsT[:], in_=psA[0][:, 0:80]), reads=["psA0"], writes=["colsT"])
        P.op("act", lambda e: e.activation(out=condb[:], in_=colsT[:, 64:80], func=AF.Silu), reads=["colsT"], writes=["condb"])
        for s in range(2):
            P.op("dve", lambda e, s=s: e.tensor_copy(out=rep[:, s, :, :], in_=condb[:, s * 8:(s + 1) * 8].unsqueeze(2).to_broadcast([128, 8, 128])),
                 reads=["condb"], writes=["rep"])
        def ada_load(j):
            sl = 2 * (j % 3)
            Wp = wslot(sl, [128, 8, 1024])
            P.dma("pool", "w%d" % sl, lambda e: e.dma_start(out=Wp, in_=w_ada[:, j * D:(j + 1) * D].rearrange("(k p) n -> p k n", p=128)),
                  writes=wres(sl, 2))
            return sl, Wp

        def ada_cols(slWp, bank, bname, c0):
            sl, Wp = slWp
            for m in range(8):
                mm(bank[:, (c0 + m) * 2:(c0 + m) * 2 + 2], [(Wp[:, k, m * 128:(m + 1) * 128], condb[:, k:16:8]) for k in range(8)],
                   reads=wres(sl, 2) + ["condb"], writes=[bname])

        def ada_rows(slWp, which):
            sl, Wp = slWp
            for s in range(2):
                for half in range(2):
                    mm(psA[half][:], [(rep[:, s, k, :], Wp[:, k, half * 512:(half + 1) * 512]) for k in range(8)],
                       reads=wres(sl, 2) + ["rep"], writes=["psA%d" % half])
                    P.op("dve", lambda e, s=s, half=half: e.tensor_tensor(
                        out=gaterow[:, s, which, half * 512:(half + 1) * 512], in0=psA[half][:],
                        in1=gbias[:, which, half * 512:(half + 1) * 512], op=ALU.add),
                        reads=["psA%d" % half, "gbias"], writes=["gaterow"])

        def ada_fin(wh, bank, bname):
            a0 = 24 * wh
            P.op("dve", lambda e: e.tensor_tensor(out=modc[:, a0:a0 + 16, :], in0=bank[:, 0:32].rearrange("p (a b) -> p a b", b=2),
                                                  in1=colsT[:, 16 + a0:32 + a0].unsqueeze(2).to_broadcast([128, 16, 2]), op=ALU.add),
                 reads=[bname, "colsT"], writes=[("modc", wh)])
            for s in range(2):
                P.op("dve", lambda e, s=s: e.scalar_tensor_tensor(
                    out=Gc[:, wh, s, :], in0=modc[:, a0 + 8:a0 + 16, s], scalar=1.0,
                    in1=colsT[:, wh * 8:(wh + 1) * 8], op0=ALU.add, op1=ALU.mult), reads=[("modc", wh), "colsT"], writes=[("Gc", wh)])
        adaL = {j: ada_load(j) for j in range(2)}
        ada_cols(adaL[0], psB[0], "psB0", 0)
        ada_cols(adaL[1], psB[0], "psB0", 8)
        ada_fin(0, psB[0], "psB0")
        for j in (2, 3, 4):
            adaL[j] = ada_load(j)

        def ada_part2():
            ada_rows(adaL[2], 0)
            ada_cols(adaL[3], psB[1], "psB1", 0)
            ada_cols(adaL[4], psB[1], "psB1", 8)
            adaL[5] = ada_load(5)
            ada_fin(1, psB[1], "psB1")
            ada_rows(adaL[5], 1)

        def shiftc(wh, s, k):
            return modc[:, 3 * wh * 8 + k, s:s + 1]

        R4A = ["xs0", "xs1", "xs2", "xnb0", "xnb1"]
        R4P = ["gbias", "wsf", "trilT", "bsf", "bsh", "rep"]
        R4B = ["accB", "lnt0", "lnt1", "rs0", "rs1"]
        R2B = ["qT", "kT", "vT", "vtok", "sq0", "sq1", "pT0", "pT1", "pT2"]
        R4C = ["gv0", "gv1", "gv2", "gv3", "vn", "ug0", "ug1"]
        R4D = ["heT0", "heT1", "sgD0", "sgD1", "stg0", "stg1"]
        xs = [view(R4, i * 4096, [128, 1024], F32) for i in range(3)]
        xnb = [view(R4, 12288 + i * 2048, [128, 1024], BF16) for i in range(2)]

        def rstd_small(ss_ap, ln_ap, rs_ap, scale, r, w):
            P.op("act", lambda e: e.activation(out=ln_ap, in_=ss_ap, func=AF.Ln, scale=scale, bias=epsT[:]), reads=r + ["epsT"], writes=[w + "_ln"])
            P.op("act", lambda e: e.activation(out=rs_ap, in_=ln_ap, func=AF.Exp, scale=-0.5), reads=[w + "_ln"], writes=[w])

        def norm_transpose(src_ap, src_res, tt, wh, s, dstT, dst_res, extra_w=(), defer=False):
            P.op("act", lambda e: e.activation(out=junk[:], in_=src_ap, func=AF.Square, accum_out=ssA[:, tt:tt + 1]),
                 reads=src_res, writes=["junk", ("ssA", tt)])
            rstd_small(ssA[:, tt:tt + 1], lnA[:, tt:tt + 1], rsA[:, tt:tt + 1], 1.0 / D, [("ssA", tt)], "rsA%d" % tt)
            xb = xnb[tt % 2]
            P.op("dve", lambda e: e.tensor_scalar(out=xb, in0=src_ap, scalar1=rsA[:, tt:tt + 1], scalar2=None, op0=ALU.mult),
                 reads=src_res + ["rsA%d" % tt], writes=["xnb%d" % (tt % 2)])
            if not defer:
                transpose_evac(tt, wh, s, dstT, dst_res, extra_w)

        def transpose_evac(tt, wh, s, dstT, dst_res, extra_w=()):
            xb = xnb[tt % 2]
            pt = psT[tt % 2]

            def tr(e):
                for k in range(8):
                    ins = e.transpose(pt[:, k, :], xb[:, k * 128:(k + 1) * 128], identb[:])
                return ins
            P.op("pe", tr, reads=["xnb%d" % (tt % 2), "identb"], writes=["psT%d" % (tt % 2)])

            def ev_act(e):
                for k in range(8):
                    ins = e.activation(out=dstT[:, k, tt * 128:(tt + 1) * 128], in_=pt[:, k, :], func=AF.Identity,
                                       scale=Gc[:, wh, s, k:k + 1], bias=shiftc(wh, s, k))
                return ins

            def ev_dve(e):
                for k in range(8):
                    ins = e.tensor_scalar(out=dstT[:, k, tt * 128:(tt + 1) * 128], in0=pt[:, k, :], scalar1=Gc[:, wh, s, k:k + 1],
                                          scalar2=shiftc(wh, s, k), op0=ALU.mult, op1=ALU.add)
                return ins
            if tt % 2 == 0:
                P.op("act", ev_act, reads=["psT%d" % (tt % 2), ("Gc", wh), ("modc", wh)], writes=[dst_res] + list(extra_w))
            else:
                P.op("dve", ev_dve, reads=["psT%d" % (tt % 2), ("Gc", wh), ("modc", wh)], writes=[dst_res] + list(extra_w))

        def seq_body(s, prevR4):
            P.alias(R4A, prevR4)
            P.alias([("hT", st) for st in range(4)], [("acc", tt, hh) for tt in range(16) for hh in range(2)])
            P.op("dve", lambda e: e.memset(ssA[:], 0.0), writes=[("ssA", i) for i in range(16)])
            for tt in range(16):
                sl = tt % 3
                P.dma("sp" if s == 0 else "act", "x%d" % sl, lambda e, sl=sl, tt=tt: e.dma_start(out=xs[sl], in_=x_d[s, tt * 128:(tt + 1) * 128, :]),
                      writes=["xs%d" % sl])
                norm_transpose(xs[sl], ["xs%d" % sl], tt, 0, s, hT, ("hT", tt // 4), defer=True)
                if tt >= 1:
                    transpose_evac(tt - 1, 0, s, hT, ("hT", (tt - 1) // 4))
            transpose_evac(15, 0, s, hT, ("hT", 3))
            add_dbg("hT%d" % s, hT, [128, 8, S], [("hT", st) for st in range(4)])
            if s == 0:
                ada_part2()

            P.alias(R4B, R4A + RTN + ["ot0", "ot1", "ot2", "rep"])
            P.alias(R2B, [("h2T", tt) for tt in range(16)] + R4D + ["gbias"])
            P.alias([("oT", h) for h in range(8)], [("acc", tt, hh) for tt in range(16) for hh in range(2)])
            qT = view(R2, 0, [128, S], BF16); kT = view(R2, 4096, [128, S], BF16); vT = view(R2, 8192, [128, S], BF16)
            vtok = view(R2, 12288, [128, 3, 16, 128], BF16)
            pT = [view(R2, 24576 + i * 1024, [128, 512], BF16) for i in range(3)]
            sqb = [view(R2, 27648 + i * 1024, [128, 512], BF16) for i in range(2)]
            accB = view(R4, 0, [128, 2, S], F32)
            lnt = [view(R4, 16384 + i * 2048, [128, 512], F32) for i in range(2)]
            rs = [view(R4, 20480 + i * 2048, [128, 512], F32) for i in range(2)]
            Sb = [(psS, "psS"), (psB[0], "psB0")]
            Ob = [(psO, "psO"), (psB[1], "psB1")]

            def load_head(h):
                sl = (s * 8 + h) % 6
                wv = wslot(sl, [128, 8, 3, 128])
                for j in range(3):
                    c0 = (2 + j) * D + h * 128
                    P.dma("pool", "w%d" % sl, lambda e, j=j, c0=c0: e.dma_start(
                        out=wv[:, :, j, :], in_=w_in[:, c0:c0 + 128].rearrange("(k p) n -> p k n", p=128)),
                        writes=wres(sl))
                return sl, wv
            def load16(slot, src):
                Wv = wslot(slot, [128, 8, 1024])
                P.dma("pool", "w%d" % slot, lambda e: e.dma_start(out=Wv, in_=src.rearrange("(k p) n -> p k n", p=128)), writes=wres(slot, 2))
                return Wv
            c4spec = {"Wgb": (0, w_in[:, 6 * D:7 * D]), "Wpb": (2, w_pb), "Wvs": (4, w_in[:, D:2 * D])}
            c4w = {}
            nxh = load_head(0)
            ci = 0
            for h in range(8):
                sl, wv = nxh
                if h + 1 < 8:
                    nxh = load_head(h + 1)
                if h == 6:
                    busy = {(s * 8 + 6) % 6, (s * 8 + 7) % 6}
                    for nm_, (slot_, src_) in c4spec.items():
                        if not ({slot_, slot_ + 1} & busy):
                            c4w[nm_] = load16(slot_, src_)
                items = [(j, st, dst, dn) for (j, dst, dn) in ((1, kT, "kT"), (2, vT, "vT"), (0, qT, "qT")) for st in range(4)]

                def post2(a, j, st, dst, dn):
                    tsl = slice(st * 512, (st + 1) * 512)
                    mm(psB[a][:], [(onesb[:], sqb[a])], reads=["onesb", "sq%d" % a], writes=["psB%d" % a])
                    P.op("act", lambda e: e.activation(out=lnt[a], in_=psB[a][:], func=AF.Ln, scale=1.0 / 128, bias=epsT[:]),
                         reads=["psB%d" % a, "epsT"], writes=["lnt%d" % a])
                    P.op("act", lambda e: e.activation(out=rs[a], in_=lnt[a], func=AF.Exp, scale=-0.5), reads=["lnt%d" % a], writes=["rs%d" % a])
                    P.op("dve", lambda e: e.scalar_tensor_tensor(out=dst[:, tsl], in0=dst[:, tsl], scalar=gqk[:, j:j + 1], in1=rs[a],
                                                                  op0=ALU.mult, op1=ALU.mult),
                         reads=[dn, "gqk", "rs%d" % a], writes=[dn])
                pend = None
                for (j, st, dst, dn) in items:
                    a = ci % 2
                    ci += 1
                    tsl = slice(st * 512, (st + 1) * 512)
                    mm(psA[a][:], [(wv[:, k, j, :], hT[:, k, tsl]) for k in range(8)],
                       reads=wres(sl) + [("hT", st)], writes=["psA%d" % a])
                    P.op("dve", lambda e, a=a, dst=dst, tsl=tsl: e.tensor_copy(out=dst[:, tsl], in_=psA[a][:]), reads=["psA%d" % a], writes=[dn])
                    if j != 2:
                        P.op("act", lambda e, a=a, dst=dst, tsl=tsl: e.activation(out=sqb[a], in_=dst[:, tsl], func=AF.Square), reads=[dn], writes=["sq%d" % a])
                    if pend is not None:
                        post2(*pend)
                    pend = (a, j, st, dst, dn) if j != 2 else None
                if pend is not None:
                    post2(*pend)
                if _STOP == 1:
                    continue
                for p in range(3):
                    for b0 in range(0, 16, 8):
                        t = (p * 2 + b0 // 8) % 2
                        pt = psT[t]

                        def trv(e, p=p, b0=b0, pt=pt):
                            for i in range(8):
                                ins = e.transpose(pt[:, i, :], vT[:, tokslice(p, b0 + i)], identb[:])
                            return ins
                        P.op("pe", trv, reads=["vT", "identb"], writes=["psT%d" % t])
                        P.op("dve", lambda e, p=p, b0=b0, pt=pt: e.tensor_copy(out=vtok[:, p, b0:b0 + 8, :], in_=pt[:, :, :]),
                             reads=["psT%d" % t], writes=["vtok"])
                if _STOP == 2:
                    continue
                units = [(p, qb0) for p in range(3) for qb0 in range(0, 16, 2)]

                def hasprev(p, qb):
                    return (p == 0 and qb > 0) or (p == 1 and qb % 4 > 0)

                def st_qk(u, p, qb0):
                    bank, bn = Sb[u % 2]
                    Sv = bank[:].rearrange("p (b h t) -> p b h t", b=2, h=2)

                    def fn(e):
                        for bl in range(2):
                            qb = qb0 + bl
                            tq = tokslice(p, qb)
                            ks = tokslice(p, qb - 1) if hasprev(p, qb) else tq
                            e.matmul(Sv[:, bl, 0, :], lhsT=kT[:, ks], rhs=qT[:, tq], start=True, stop=True)
                            ins = e.matmul(Sv[:, bl, 1, :], lhsT=kT[:, tq], rhs=qT[:, tq], start=True, stop=True)
                        return ins
                    P.op("pe", fn, reads=["kT", "qT"], writes=[bn])
                    pi = u % 3
                    mi = 2 if p == 2 else (1 if not hasprev(p, qb0) else 0)
                    P.op("act", lambda e: e.activation(out=pT[pi], in_=bank[:], func=AF.Exp), reads=[bn], writes=["pT%d" % pi])
                    P.op("pool", lambda e: e.tensor_tensor(out=pT[pi], in0=pT[pi], in1=amask[:, mi, :], op=ALU.mult),
                         reads=["pT%d" % pi, "amask"], writes=["pT%d" % pi])

                def st_pv(u, p, qb0):
                    bank, bn = Ob[u % 2]
                    Ov = bank[:].rearrange("p (b o t) -> p b o t", b=2, o=2)
                    pi = u % 3

                    def fn(e):
                        for bl in range(2):
                            qb = qb0 + bl
                            hp = hasprev(p, qb)
                            for o, lh in ((0, None), (1, onesb[:])):
                                if hp:
                                    e.matmul(Ov[:, bl, o, :], lhsT=(vtok[:, p, qb - 1, :] if o == 0 else lh), rhs=pT[pi][:, bl * 256:bl * 256 + 128],
                                             start=True, stop=False)
                                ins = e.matmul(Ov[:, bl, o, :], lhsT=(vtok[:, p, qb, :] if o == 0 else lh), rhs=pT[pi][:, bl * 256 + 128:bl * 256 + 256],
                                               start=(not hp), stop=True)
                        return ins
                    P.op("pe", fn, reads=["vtok", "onesb", "pT%d" % pi], writes=[bn])
                    if p == 0:
                        dv = accB[:, :, qb0 * 128:qb0 * 128 + 256].rearrange("p o (b t) -> p b o t", b=2)
                    elif p == 1:
                        st0 = (qb0 // 4) + 512 * (qb0 % 4)
                        dv = accB[:, :, st0:st0 + 1021:4].rearrange("p o (b t) -> p b o t", b=2)
                    else:
                        dv = accB[:, :, :].rearrange("p o (i r) -> p r o i", r=16)[:, qb0:qb0 + 2, :, :]
                    if p == 0:
                        P.op("dve", lambda e: e.tensor_copy(out=dv, in_=Ov), reads=[bn], writes=["accB"])
                    else:
                        P.op("dve", lambda e: e.tensor_tensor(out=dv, in0=Ov, in1=dv, op=ALU.add), reads=[bn, "accB"], writes=["accB"])
                pend_u = []
                for u, (p, qb0) in enumerate(units):
                    st_qk(u, p, qb0)
                    pend_u.append((u, p, qb0))
                    if len(pend_u) > 2:
                        st_pv(*pend_u.pop(0))
                while pend_u:
                    st_pv(*pend_u.pop(0))
                for st in range(4):
                    a = st % 2
                    P.op("act", lambda e, a=a, st=st: e.activation(out=lnt[a], in_=accB[:, 1, st * 512:(st + 1) * 512], func=AF.Ln),
                         reads=["accB"], writes=["lnt%d" % a])
                    P.op("act", lambda e, a=a: e.activation(out=rs[a], in_=lnt[a], func=AF.Exp, scale=-1.0), reads=["lnt%d" % a], writes=["rs%d" % a])
                    P.op("dve", lambda e, a=a, st=st, h=h: e.tensor_tensor(out=oT[:, h, st * 512:(st + 1) * 512], in0=accB[:, 0, st * 512:(st + 1) * 512],
                                                                         in1=rs[a], op=ALU.mult),
                         reads=["accB", "rs%d" % a], writes=[("oT", h)])
            add_dbg("oT%d" % s, oT, [128, 8, S], [("oT", h) for h in range(8)])
            if _STOP:
                return R4B

            P.alias(R4C + ["sg0", "sg1", "tmpc0", "tmpc1"], R4B)
            P.alias([("mg", st) for st in range(4)], R2B)
            sg = [view(R4, 24576 + i * 2048, [128, 512], F32) for i in range(2)]
            gv = [view(R4, i * 4096, [128, 1024], F32) for i in range(4)]
            vn = view(R4, 16384, [128, 4, 1024], BF16)
            ug = sg

            for nm_ in ("Wgb", "Wpb", "Wvs"):
                if nm_ not in c4w:
                    c4w[nm_] = load16(*c4spec[nm_])
            Wgb, Wpb, Wvs = c4w["Wgb"], c4w["Wpb"], c4w["Wvs"]
            ci = 0
            for st in range(4):
                tsl = slice(st * 512, (st + 1) * 512)
                for ct in range(8):
                    a = ci % 2
                    ci += 1
                    csl = slice(ct * 128, (ct + 1) * 128)
                    mm(psA[a][:], [(Wgb[:, k, csl], hT[:, k, tsl]) for k in range(8)], reads=wres(0, 2) + [("hT", st)], writes=["psA%d" % a])
                    mm(psB[a][:], [(Wpb[:, k, csl], oT[:, k, tsl]) for k in range(8)], reads=wres(2, 2) + [("oT", k) for k in range(8)], writes=["psB%d" % a])
                    P.op("act", lambda e, a=a: e.activation(out=sg[a], in_=psA[a][:], func=AF.Sigmoid), reads=["psA%d" % a], writes=["sg%d" % a])
                    P.op("dve", lambda e, a=a, ct=ct, tsl=tsl: e.tensor_tensor(out=mg[:, ct, tsl], in0=psB[a][:], in1=sg[a], op=ALU.mult),
                         reads=["psB%d" % a, "sg%d" % a], writes=[("mg", st)])
            P.alias([("zT", st) for st in range(4)], [("oT", h) for h in range(8)])
            Wu = load16(0, w_in[:, 0:D])
            Wga = load16(2, w_in[:, 5 * D:6 * D])
            for st in range(4):
                tsl = slice(st * 512, (st + 1) * 512)
                P.op("dve", lambda e: e.memset(ssV[:], 0.0), writes=[("ssV", i) for i in range(4)])
                for t4 in range(4):
                    tt = st * 4 + t4
                    for half in range(2):
                        mm(psA[half][:], [(hT[:, k, tt * 128:(tt + 1) * 128], Wvs[:, k, half * 512:(half + 1) * 512]) for k in range(8)],
                           reads=wres(4, 2) + [("hT", st)], writes=["psA%d" % half])
                        P.op("act", lambda e, half=half, t4=t4: e.activation(out=gv[t4][:, half * 512:(half + 1) * 512], in_=psA[half][:], func=AF.Gelu_apprx_tanh),
                             reads=["psA%d" % half], writes=["gv%d" % t4])
                    P.op("act", lambda e, t4=t4: e.activation(out=junk[:], in_=gv[t4], func=AF.Square, accum_out=ssV[:, t4:t4 + 1]),
                         reads=["gv%d" % t4], writes=["junk", ("ssV", t4)])
                rstd_small(ssV[:], lnV[:], rsV[:], 1.0 / D, [("ssV", i) for i in range(4)], "rsV")
                for t4 in range(4):
                    P.op("dve", lambda e, t4=t4: e.scalar_tensor_tensor(out=vn[:, t4, :], in0=gv[t4], scalar=rsV[:, t4:t4 + 1], in1=gsgu[:], op0=ALU.mult, op1=ALU.mult),
                         reads=["gv%d" % t4, "rsV", "gsgu"], writes=["vn"])
                for g in range(8):
                    a = g % 2
                    csl = slice(g * 128, (g + 1) * 128)
                    mm(psB[a][:], [(Wu[:, k, csl], hT[:, k, tsl]) for k in range(8)], reads=wres(0, 2) + [("hT", st)], writes=["psB%d" % a])
                    P.op("act", lambda e, a=a: e.activation(out=ug[a], in_=psB[a][:], func=AF.Gelu_apprx_tanh), reads=["psB%d" % a], writes=["sg%d" % a])
                    pm = psS if a == 0 else psO
                    pmn = "psS" if a == 0 else "psO"
                    pmn2 = pmn

                    def mix(e, g=g, pm=pm, csl=csl):
                        for t4 in range(4):
                            e.matmul(pm[:, t4 * 128:(t4 + 1) * 128], lhsT=vn[:, t4, csl], rhs=WsT[:, g, :], start=True, stop=False)
                            ins = e.matmul(pm[:, t4 * 128:(t4 + 1) * 128], lhsT=onesb[0:33, :], rhs=bs2[0:33, csl], start=False, stop=True)
                        return ins
                    P.op("pe", mix, reads=["vn", "WsT", "onesb"] + BS2, writes=[pmn, pmn2])
                    P.op("dve", lambda e, a=a, g=g, pm=pm, tsl=tsl: e.tensor_tensor(out=zT[:, g, tsl], in0=pm[:], in1=ug[a], op=ALU.mult),
                         reads=[pmn, pmn2, "sg%d" % a], writes=[("zT", st)])
            add_dbg("zT%d" % s, zT, [128, 8, S], [("zT", st) for st in range(4)])
            Wpa = load16(4, w_pa)
            Wo = load16(0, w_out)
            tmpc = [view(R4, i * 2048, [128, 512], F32) for i in range(2)]
            P.alias(["tmpc0", "tmpc1"], ["gv0"])
            ci = 0
            for st in range(4):
                tsl = slice(st * 512, (st + 1) * 512)
                for ct in range(8):
                    a = ci % 2
                    ci += 1
                    csl = slice(ct * 128, (ct + 1) * 128)
                    mm(psA[a][:], [(Wga[:, k, csl], hT[:, k, tsl]) for k in range(8)], reads=wres(2, 2) + [("hT", st)], writes=["psA%d" % a])
                    mm(psB[a][:], [(Wpa[:, k, csl], zT[:, k, tsl]) for k in range(8)], reads=wres(4, 2) + [("zT", st)], writes=["psB%d" % a])
                    P.op("act", lambda e, a=a: e.activation(out=sg[a], in_=psA[a][:], func=AF.Sigmoid), reads=["psA%d" % a], writes=["sg%d" % a])
                    P.op("dve", lambda e, a=a: e.tensor_tensor(out=tmpc[a], in0=psB[a][:], in1=sg[a], op=ALU.mult),
                         reads=["psB%d" % a, "sg%d" % a], writes=["tmpc%d" % a])
                    P.op("pool", lambda e, a=a, ct=ct, tsl=tsl: e.tensor_tensor(out=mg[:, ct, tsl], in0=tmpc[a], in1=mg[:, ct, tsl], op=ALU.add),
                         reads=["tmpc%d" % a, ("mg", st)], writes=[("mg", st)])
            add_dbg("mg%d" % s, mg, [128, 8, S], [("mg", st) for st in range(4)])
            R4E = ["xs0", "xs1", "xs2", "xnb0", "xnb1"]
            P.alias(R4E + RTN, R4C + ["sg0", "sg1", "tmpc0", "tmpc1"])
            P.alias([("acc", tt, hh) for tt in range(16) for hh in range(2)], [("hT", st) for st in range(4)] + [("zT", st) for st in range(4)])
            P.op("dve", lambda e: e.memset(ssA[:], 0.0), writes=[("ssA", i) for i in range(16)])
            P.op("dve", lambda e: e.tensor_tensor(out=Wo, in0=Wo, in1=gaterow[:, s, 0, :].unsqueeze(1).to_broadcast([128, 8, 1024]), op=ALU.mult),
                 reads=wres(0, 2) + ["gaterow"], writes=wres(0, 2))
            for tt in range(16):
                sl = tt % 3
                P.dma("sp", "x%d" % sl, lambda e, sl=sl, tt=tt: e.dma_start(out=xs[sl], in_=x_d[s, tt * 128:(tt + 1) * 128, :]),
                      writes=["xs%d" % sl])
                for half in range(2):
                    hs = slice(half * 512, (half + 1) * 512)
                    mm(psA[half][:], [(mg[:, k, tt * 128:(tt + 1) * 128], Wo[:, k, hs]) for k in range(8)],
                       reads=wres(0, 2) + [("mg", tt // 4), ("mgt", tt)], writes=["psA%d" % half])
                    P.op("dve", lambda e, half=half, hs=hs, tt=tt, sl=sl: e.tensor_tensor(out=acc[:, tt, hs], in0=psA[half][:], in1=xs[sl][:, hs], op=ALU.add),
                         reads=["psA%d" % half, "xs%d" % sl], writes=[("acc", tt, half)])
                norm_transpose(acc[:, tt, :], [("acc", tt, 0), ("acc", tt, 1)], tt, 1, s, h2T, ("h2T", tt), extra_w=[("mgt", tt)], defer=True)

                def c5_stage2(tt):
                    transpose_evac(tt, 1, s, h2T, ("h2T", tt), [("mgt", tt)])
                if tt >= 1:
                    c5_stage2(tt - 1)
            c5_stage2(15)
            lgv = psS[:, 0:320].rearrange("p (t n) -> p t n", n=20)

            def lg(e):
                for tt in range(16):
                    for k in range(8):
                        ins = e.matmul(lgv[:, tt, :], lhsT=h2T[:, k, tt * 128:(tt + 1) * 128], rhs=Wr[:, k, :], start=(k == 0), stop=(k == 7))
                return ins
            P.op("pe", lg, reads=[("h2T", tt) for tt in range(16)] + ["Wr"], writes=["psS"])
            P.op("dve", lambda e: e.tensor_tensor(out=logits[:], in0=lgv, in1=brow[:].unsqueeze(1).to_broadcast([128, 16, 20]), op=ALU.add),
                 reads=["psS", "brow"], writes=["logits"])
            add_dbg("x1_%d" % s, acc, [128, 16, D], [("acc", tt, hh) for tt in range(16) for hh in range(2)])
            add_dbg("h2T%d" % s, h2T, [128, 8, S], [("h2T", tt) for tt in range(16)])
            add_dbg("logits%d" % s, logits[:], [128, 16, 20], ["logits"])
            Lg = logits[:, :, 0:4]
            Le = logits[:, :, 4:20].rearrange("p t (g e) -> p t g e", e=4)

            def bc3(ap):
                return ap.unsqueeze(2).to_broadcast([128, 16, 4])

            def dv(fn, r, w):
                P.op("dve", fn, reads=["rt_" + n if n in rt else n for n in r], writes=["rt_" + n if n in rt else n for n in w])
            R = rt
            dv(lambda e: e.tensor_reduce(out=R["gmax"], in_=Lg, axis=AX.X, op=ALU.max), ["logits"], ["gmax"])
            dv(lambda e: e.tensor_tensor(out=R["ohg"], in0=Lg, in1=bc3(R["gmax"]), op=ALU.is_equal), ["logits", "gmax"], ["ohg"])
            dv(lambda e: e.tensor_copy(out=R["m1"], in_=R["ohg"][:, :, 0]), ["ohg"], ["m1"])
            for g_ in range(1, 4):
                dv(lambda e: e.tensor_scalar(out=R["m2"], in0=R["m1"], scalar1=-1.0, scalar2=1.0, op0=ALU.mult, op1=ALU.add), ["m1"], ["m2"])
                dv(lambda e, g_=g_: e.tensor_tensor(out=R["ohg"][:, :, g_], in0=R["ohg"][:, :, g_], in1=R["m2"], op=ALU.mult), ["ohg", "m2"], ["ohg"])
                if g_ < 3:
                    dv(lambda e, g_=g_: e.tensor_tensor(out=R["m1"], in0=R["m1"], in1=R["ohg"][:, :, g_], op=ALU.max), ["m1", "ohg"], ["m1"])
            dv(lambda e: e.tensor_tensor(out=R["dg"], in0=Lg, in1=bc3(R["gmax"]), op=ALU.subtract), ["logits", "gmax"], ["dg"])
            P.op("act", lambda e: e.activation(out=R["eg"], in_=R["dg"], func=AF.Exp), reads=["rt_dg"], writes=["rt_eg"])
            dv(lambda e: e.tensor_reduce(out=R["sumg"], in_=R["eg"], axis=AX.X, op=ALU.add), ["eg"], ["sumg"])
            dv(lambda e: e.reciprocal(out=R["pg"], in_=R["sumg"]), ["sumg"], ["pg"])
            dv(lambda e: e.tensor_tensor(out=R["tmp"], in0=Le, in1=R["ohg"].unsqueeze(3).to_broadcast([128, 16, 4, 4]), op=ALU.mult), ["logits", "ohg"], ["tmp"])
            dv(lambda e: e.tensor_reduce(out=R["sel"], in_=R["tmp"].rearrange("p t g e -> p t e g"), axis=AX.X, op=ALU.add), ["tmp"], ["sel"])
            dv(lambda e: e.tensor_reduce(out=R["m1"], in_=R["sel"], axis=AX.X, op=ALU.max), ["sel"], ["m1"])
            dv(lambda e: e.tensor_tensor(out=R["oh1"], in0=R["sel"], in1=bc3(R["m1"]), op=ALU.is_equal), ["sel", "m1"], ["oh1"])
            dv(lambda e: e.scalar_tensor_tensor(out=R["sel2"], in0=R["oh1"], scalar=-1e30, in1=R["sel"], op0=ALU.mult, op1=ALU.add), ["oh1", "sel"], ["sel2"])
            dv(lambda e: e.tensor_reduce(out=R["m2"], in_=R["sel2"], axis=AX.X, op=ALU.max), ["sel2"], ["m2"])
            dv(lambda e: e.tensor_tensor(out=R["oh2"], in0=R["sel2"], in1=bc3(R["m2"]), op=ALU.is_equal), ["sel2", "m2"], ["oh2"])
            dv(lambda e: e.tensor_tensor(out=R["d"], in0=R["m2"], in1=R["m1"], op=ALU.subtract), ["m2", "m1"], ["d"])
            P.op("act", lambda e: e.activation(out=R["ed"], in_=R["d"], func=AF.Exp), reads=["rt_d"], writes=["rt_ed"])
            dv(lambda e: e.tensor_scalar(out=R["den"], in0=R["ed"], scalar1=1.0, scalar2=None, op0=ALU.add), ["ed"], ["den"])
            dv(lambda e: e.reciprocal(out=R["r"], in_=R["den"]), ["den"], ["r"])
            dv(lambda e: e.tensor_tensor(out=R["p1"], in0=R["r"], in1=R["pg"], op=ALU.mult), ["r", "pg"], ["p1"])
            dv(lambda e: e.tensor_tensor(out=R["p2"], in0=R["ed"], in1=R["p1"], op=ALU.mult), ["ed", "p1"], ["p2"])
            dv(lambda e: e.tensor_tensor(out=R["c1"], in0=R["oh1"], in1=bc3(R["p1"]), op=ALU.mult), ["oh1", "p1"], ["c1"])
            dv(lambda e: e.tensor_tensor(out=R["c2"], in0=R["oh2"], in1=bc3(R["p2"]), op=ALU.mult), ["oh2", "p2"], ["c2"])
            dv(lambda e: e.tensor_tensor(out=R["cwg"], in0=R["c1"], in1=R["c2"], op=ALU.add), ["c1", "c2"], ["cwg"])
            cw4 = cw[:, :, 0:16].rearrange("p t (g e) -> p t g e", e=4)
            dv(lambda e: e.tensor_tensor(out=cw4, in0=R["ohg"].unsqueeze(3).to_broadcast([128, 16, 4, 4]),
                                         in1=R["cwg"].unsqueeze(2).to_broadcast([128, 16, 4, 4]), op=ALU.mult), ["ohg", "cwg"], ["cw"])
            dv(lambda e: e.tensor_copy(out=cw[:, :, 16], in_=rsA[:, 0:16]), ["rsA%d" % i for i in range(16)] + ["cw"], ["cw"])
            add_dbg("cw%d" % s, cw[:, :, 0:16], [128, 16, 16], ["cw"])

            dv(lambda e: e.tensor_copy(out=ohb[:], in_=R["ohg"]), ["ohg"], ["ohb"])
            cntv = psS[:, 0:64].rearrange("p (t g) -> p t g", g=4)

            def cums(e):
                for i in range(16):
                    for j in range(i):
                        e.matmul(cntv[:, i, :], lhsT=onesb[:], rhs=ohb[:, j, :], start=(j == 0), stop=False)
                    ins = e.matmul(cntv[:, i, :], lhsT=utri[:], rhs=ohb[:, i, :], start=(i == 0), stop=True)
                return ins
            P.op("pe", cums, reads=["ohb", "onesb", "utri"], writes=["psS"])
            mm(psO[:, 0:4], [(onesb[:], ohb[:, j, :]) for j in range(16)], reads=["ohb", "onesb"], writes=["psO"])
            dv(lambda e: e.tensor_copy(out=tot[:], in_=psO[:, 0:4]), ["psO"], ["tot"])
            dv(lambda e: e.tensor_copy(out=cnt3[:], in_=cntv), ["psS"], ["cnt3"])
            dv(lambda e: e.memset(stt[:], -1.0), [], ["stt"])
            for g in range(1, 4):
                dv(lambda e, g=g: e.tensor_tensor(out=stt[:, g:g + 1], in0=stt[:, g - 1:g], in1=tot[:, g - 1:g], op=ALU.add), ["stt", "tot"], ["stt"])
            dv(lambda e: e.tensor_tensor(out=ent[:], in0=stt[:], in1=tot[:], op=ALU.add), ["stt", "tot"], ["ent"])
            dv(lambda e: e.tensor_tensor(out=cnt3[:], in0=cnt3[:], in1=stt[:].unsqueeze(1).to_broadcast([128, 16, 4]), op=ALU.add), ["cnt3", "stt"], ["cnt3"])
            dv(lambda e: e.tensor_tensor(out=cnt3[:], in0=cnt3[:], in1=R["ohg"], op=ALU.mult), ["cnt3", "ohg"], ["cnt3"])
            dv(lambda e: e.tensor_reduce(out=posf[:], in_=cnt3[:], axis=AX.X, op=ALU.add), ["cnt3"], ["posf"])
            dv(lambda e: e.tensor_copy(out=posi[:], in_=posf[:]), ["posf"], ["posi"])
            dv(lambda e: e.tensor_tensor(out=flA[:], in0=stt[:].unsqueeze(2).to_broadcast([128, 4, 8]),
                                         in1=blkc[:, 0, :].unsqueeze(1).to_broadcast([128, 4, 8]), op=ALU.is_lt), ["stt", "blkc"], ["flA"])
            dv(lambda e: e.tensor_tensor(out=flB[:], in0=ent[:].unsqueeze(2).to_broadcast([128, 4, 8]),
                                         in1=blkc[:, 1, :].unsqueeze(1).to_broadcast([128, 4, 8]), op=ALU.is_ge), ["ent", "blkc"], ["flB"])
            dv(lambda e: e.tensor_tensor(out=flA[:], in0=flA[:], in1=flB[:], op=ALU.mult), ["flA", "flB"], ["flA"])
            dv(lambda e: e.tensor_copy(out=flags_i[:], in_=flA[:].rearrange("p g b -> p (g b)")), ["flA"], ["flags"])
            dv(lambda e: e.tensor_reduce(out=flQ[:], in_=flA[:].rearrange("p g (q b) -> p g q b", b=4), axis=AX.X, op=ALU.max), ["flA"], ["flQ"])
            dv(lambda e: e.tensor_copy(out=flags_q[:], in_=flQ[:].rearrange("p g q -> p (g q)")), ["flQ"], ["flags"])
            add_dbg("posf%d" % s, posf[:], [128, 16], ["posf"])
            add_dbg("flA%d" % s, flA[:], [128, 4, 8], ["flA"])
            for tt in range(16):
                idx = bass.IndirectOffsetOnAxis(ap=posi[:, tt:tt + 1], axis=0)
                P.dma("pool", "sc1", lambda e, tt=tt, idx=idx: e.indirect_dma_start(out=X1_d[s], out_offset=idx, in_=acc[:, tt, :], in_offset=None),
                      reads=[("acc", tt, 0), ("acc", tt, 1), "posi"], writes=["X1"])
                P.dma("pool", "scc", lambda e, tt=tt, idx=idx: e.indirect_dma_start(out=CW_d[s], out_offset=idx, in_=cw[:, tt, :], in_offset=None),
                      reads=["cw", "posi"], writes=["CW"])
            P.dma("sp", "rbc", lambda e: e.dma_start(out=cw[:], in_=CW_d[s].rearrange("(t p) e -> p t e", p=128)), reads=["CW"], writes=["cw"])
            for j in range(16):
                P.dma("sp", "rb%d" % j, lambda e, j=j: e.dma_start(out=acc[:, j, :], in_=X1_d[s][j * 128:(j + 1) * 128, :]),
                      reads=["X1"], writes=[("acc", j, 0), ("acc", j, 1)])
            for j in range(16):
                P.op("dve", lambda e, j=j: e.tensor_scalar(out=xnb[j % 2], in0=acc[:, j, :], scalar1=cw[:, j, 16:17], scalar2=None, op0=ALU.mult),
                     reads=[("acc", j, 0), ("acc", j, 1), "cw"], writes=["xnb%d" % (j % 2)])
                transpose_evac(j, 1, s, h2T, ("h2T", j))
            P.alias(R4D, R4E)
            heT = [view(R4, i * 4096, [128, 4, 512], BF16) for i in range(2)]
            sgD = [view(R4, 8192 + i * 2048, [128, 512], F32) for i in range(2)]
            psY = [psS, psO]
            psYn = [["psS"], ["psO"]]

            stg = [view(R4, 12288 + i * 8192, [128, 2048], F32) for i in range(2)]
            stgc = [0]

            def load_expert(e_):
                b = (3 * e_) % 6
                Wg = wslot(b, [128, 8, 512]); Wu_ = wslot((b + 1) % 6, [128, 8, 512]); Wd = wslot((b + 2) % 6, [128, 4, 1024])
                dmas, casts = [], []
                for mi, (Wdst, src) in enumerate(((Wg, w_gate), (Wu_, w_up), (Wd, w_down))):
                    for hh in range(2):
                        si = stgc[0] % 2
                        stgc[0] += 1
                        sres = "stg%d" % si
                        if mi < 2:
                            sv = stg[si].rearrange("p (k n) -> p k n", n=512)
                            srcap = src[e_, hh * 512:(hh + 1) * 512, :].rearrange("(k p) n -> p k n", p=128)
                            dstap = Wdst[:, hh * 4:(hh + 1) * 4, :]
                        else:
                            sv = stg[si].rearrange("p (k n) -> p k n", n=1024)
                            srcap = src[e_, hh * 256:(hh + 1) * 256, :].rearrange("(k p) n -> p k n", p=128)
                            dstap = Wdst[:, hh * 2:(hh + 1) * 2, :]
                        dmas.append(lambda sres=sres, sv=sv, srcap=srcap: P.dma(
                            "sp", sres, lambda e: e.dma_start(out=sv, in_=srcap), writes=[sres]))
                        if mi < 2:
                            casts.append(lambda sres=sres, sv=sv, dstap=dstap, mi=mi: P.op(
                                "pool", lambda e: e.tensor_tensor(out=dstap, in0=sv, in1=ones1[:, 0:1].unsqueeze(2).to_broadcast([128, 4, 512]), op=ALU.mult),
                                reads=[sres, "ones1"], writes=wres(b + mi)))
                        else:
                            casts.append(lambda sres=sres, sv=sv, dstap=dstap, mi=mi: P.op("pool", lambda e: e.tensor_tensor(
                                out=dstap, in0=sv, in1=gaterow[:, s, 1, :].unsqueeze(1).to_broadcast([128, 2, 1024]), op=ALU.mult),
                                reads=[sres, "gaterow"], writes=wres(b + mi)))
                d, c = dmas, casts
                steps = [d[0], d[1], c[0], d[2], c[1], d[3], c[2], d[4], c[3], d[5], c[4], c[5]]
                return (Wg, Wu_, Wd, b), steps
            nxt, steps0 = load_expert(0)
            for st_ in steps0:
                st_()
            ci = 0
            yi = 0
            hbi = 0
            for e_ in range(N_EXP):
                Wg, Wu_, Wd, b = nxt
                rest = []
                if e_ + 1 < N_EXP:
                    nxt, rest = load_expert(e_ + 1)
                    rest = list(rest)
                    while rest:
                        rest.pop(0)()
                sched = [2, 1, 1, 1, 1, 1, 1, 2]
                g = e_ // 4
                for blk in range(4):
                    P.cond_begin(flags_i[0:1, g * 8 + blk:g * 8 + blk + 1], "flags")
                    tsl = slice(blk * 512, (blk + 1) * 512)
                    hb = hbi % 2
                    hbi += 1
                    h2r = [("h2T", blk * 4 + i) for i in range(4)]
                    for f in range(4):
                        a = ci % 2
                        ci += 1
                        fsl = slice(f * 128, (f + 1) * 128)
                        mm(psA[a][:], [(Wg[:, k, fsl], h2T[:, k, tsl]) for k in range(8)], reads=wres(b) + h2r, writes=["psA%d" % a])
                        mm(psB[a][:], [(Wu_[:, k, fsl], h2T[:, k, tsl]) for k in range(8)], reads=wres(b + 1) + h2r, writes=["psB%d" % a])
                        P.op("act", lambda e, a=a: e.activation(out=sgD[a], in_=psA[a][:], func=AF.Silu), reads=["psA%d" % a], writes=["sgD%d" % a])
                        P.op("dve", lambda e, a=a, hb=hb, f=f: e.tensor_tensor(out=heT[hb][:, f, :], in0=psB[a][:], in1=sgD[a], op=ALU.mult),
                             reads=["psB%d" % a, "sgD%d" % a], writes=["heT%d" % hb])
                    for t4 in range(4):
                        tt = blk * 4 + t4
                        for half in range(2):
                            y = yi % 2
                            yi += 1
                            hs = slice(half * 512, (half + 1) * 512)
                            mm(psY[y][:], [(heT[hb][:, f, t4 * 128:(t4 + 1) * 128], Wd[:, f, hs]) for f in range(4)],
                               reads=wres(b + 2) + ["heT%d" % hb], writes=psYn[y])
                            P.op("dve", lambda e, y=y, tt=tt, hs=hs, e_=e_: e.scalar_tensor_tensor(
                                out=acc[:, tt, hs], in0=psY[y][:], scalar=cw[:, tt, e_:e_ + 1], in1=acc[:, tt, hs], op0=ALU.mult, op1=ALU.add),
                                reads=psYn[y] + ["cw", ("acc", tt, half)], writes=[("acc", tt, half)])
                    P.cond_end()
            ot = [view(R4, 16384 + i * 4096, [128, 1024], F32) for i in range(3)]
            OTN = ["ot0", "ot1", "ot2"]
            P.alias(OTN, R4D + R4E + RTN)
            for j in range(16):
                P.dma("sp", "sy", lambda e, j=j: e.dma_start(out=Y_d[s][j * 128:(j + 1) * 128, :], in_=acc[:, j, :]), reads=[("acc", j, 0), ("acc", j, 1)], writes=["Y"])
            for tt in range(16):
                o = tt % 3
                idx = bass.IndirectOffsetOnAxis(ap=posi[:, tt:tt + 1], axis=0)
                P.dma("pool", "gy%d" % o, lambda e, o=o, idx=idx: e.indirect_dma_start(out=ot[o], out_offset=None, in_=Y_d[s], in_offset=idx),
                      reads=["Y", "posi"], writes=[OTN[o]])
                P.dma("sp", "st%d" % tt, lambda e, tt=tt, o=o: e.dma_start(out=y_d[s, tt * 128:(tt + 1) * 128, :], in_=ot[o]),
                      reads=[OTN[o]], final=True)
            return R4D + R4E
        prevR4 = R4P
        for s_ in range(nseq):
            prevR4 = seq_body(s_, prevR4)
        P.emit()
    return nc


_CACHE = {}


def _consts():
    tril = np.tril(np.ones((128, 128), np.float32))
    trilT = np.ascontiguousarray(tril.T)
    tk = np.arange(128)[:, None]
    tq = np.arange(128)[None, :]
    pm = (tk >= tq).astype(np.float32); cm = (tk <= tq).astype(np.float32); z = np.zeros_like(pm)
    amask = np.stack([np.concatenate([pm, cm, pm, cm], 1), np.concatenate([z, cm, pm, cm], 1), np.concatenate([z, cm, z, cm], 1)], 1)
    amask = np.ascontiguousarray(amask).astype(ml_dtypes.bfloat16)
    blk = np.arange(8, dtype=np.float32) * 512
    blkc = np.ascontiguousarray(np.broadcast_to(np.stack([blk + 511, blk], 0)[None], (128, 2, 8))).astype(np.float32)
    return {
        "utri": np.triu(np.ones((128, 128), np.float32)).astype(ml_dtypes.bfloat16),
        "blkc": blkc,
        "identb": np.eye(128, dtype=np.float32).astype(ml_dtypes.bfloat16),
        "identf": np.eye(128, dtype=np.float32),
        "trilT": trilT,
        "amask": amask,
    }


def make_in_maps(inp, n_cores=8):
    f = lambda a: np.ascontiguousarray(np.asarray(a, dtype=np.float32))
    w_r = np.concatenate([f(inp["w_router_group"])[0]] + [f(inp["w_router_expert"])[0, g] for g in range(4)], axis=1)
    b_r = np.concatenate([f(inp["b_router_group"])[0].reshape(-1), f(inp["b_router_expert"])[0].reshape(-1)])
    shared = {
        "w_ada": f(inp["w_ada"])[0], "b_ada": f(inp["b_ada"])[0], "norm1_g": f(inp["norm1_g"])[0],
        "w_in": f(inp["w_in"])[0], "sgu_norm_g": f(inp["sgu_norm_g"])[0],
        "sgu_wT": np.ascontiguousarray(f(inp["sgu_w"])[0].transpose(0, 2, 1)),
        "sgu_b": f(inp["sgu_b"])[0].reshape(-1), "q_norm_g": f(inp["q_norm_g"])[0], "k_norm_g": f(inp["k_norm_g"])[0],
        "w_proj_a": f(inp["w_proj_a"])[0], "w_proj_b": f(inp["w_proj_b"])[0], "w_out": f(inp["w_out"])[0],
        "norm2_g": f(inp["norm2_g"])[0], "w_r": np.ascontiguousarray(w_r), "b_r": np.ascontiguousarray(b_r),
        "w_gate": f(inp["w_gate"])[0], "w_up": f(inp["w_up"])[0], "w_down": f(inp["w_down"])[0],
    }
    shared.update(_consts())
    x = f(inp["x"]); c = f(inp["c"])
    maps = []
    for i in range(n_cores):
        m = dict(shared)
        m["x"] = np.ascontiguousarray(x[NSEQ * i:NSEQ * (i + 1)])
        m["c"] = np.ascontiguousarray(c[NSEQ * i:NSEQ * (i + 1)])
        maps.append(m)
    return maps


def kernel(**inputs):
    if "nc" not in _CACHE:
        _CACHE["nc"] = build()
    nc = _CACHE["nc"]
    maps = make_in_maps(inputs)
    res = run_bass_kernel_spmd(nc, maps, core_ids=list(range(8)))
    return np.concatenate([r["y"] for r in res.results], axis=0).astype(np.float32)
```

```python
import contextlib
import numpy as np
import ml_dtypes
import concourse.bass as bass
import concourse.mybir as mybir
from concourse.bass_utils import run_bass_kernel_spmd

F32 = mybir.dt.float32
BF16 = mybir.dt.bfloat16
AF = mybir.ActivationFunctionType
ALU = mybir.AluOpType
AX = mybir.AxisListType

NSEQ = 2
_STOP = 0
S = 2048
D = 1024
EPS = 1e-6
N_EXP = 16


class Prog:
    ENGS = ("pe", "act", "dve", "pool", "sp")

    def __init__(self, nc):
        self.nc = nc
        self.ops = {e: [] for e in self.ENGS}
        self.cnt = {}
        self.seen = {e: {} for e in self.ENGS}
        self.res = {}
        self.sems = {}
        self.final = []
        self.cond_stack = []
        self.cond_first = {}
        self.ncond = 0

    def cond_begin(self, flag_ap, flag_res):
        self.ncond += 1
        self.cond_stack.append((self.ncond, flag_ap, flag_res))

    def cond_end(self):
        self.cond_stack.pop()

    def _deps(self, eng, reads, writes):
        deps = {}

        def addall(d):
            for s, v in d.items():
                if deps.get(s, 0) < v:
                    deps[s] = v
        for r in reads:
            st = self.res.get(r)
            if st is not None:
                addall(st[0])
        for w in writes:
            st = self.res.get(w)
            if st is not None:
                addall(st[0])
                addall(st[1])
        waits = []
        for s, v in deps.items():
            if s == eng and eng == "pe":
                continue
            if self.seen[eng].get(s, 0) >= v:
                continue
            self.seen[eng][s] = v
            waits.append((s, v))
        return waits

    def _commit(self, ev, reads, writes):
        s, v = ev
        for r in reads:
            st = self.res.setdefault(r, [{}, {}])
            st[1][s] = max(st[1].get(s, 0), v)
        for w in writes:
            st = self.res.setdefault(w, [{}, {}])
            st[0][s] = max(st[0].get(s, 0), v)

    def alias(self, new, old):
        ev = {}
        for o in old:
            st = self.res.get(o)
            if st is None:
                continue
            for d in st:
                for s, v in d.items():
                    ev[s] = max(ev.get(s, 0), v)
        for n in new:
            st = self.res.setdefault(n, [{}, {}])
            for s, v in ev.items():
                st[0][s] = max(st[0].get(s, 0), v)

    def op(self, eng, fn, reads=(), writes=()):
        cond = None
        if self.cond_stack:
            for (cid, flag_ap, flag_res) in self.cond_stack:
                if (cid, eng) not in self.cond_first:
                    self.cond_first[(cid, eng)] = self._deps(eng, [flag_res], [])
            saved = dict(self.seen[eng])
            waits = self._deps(eng, reads, writes)
            self.seen[eng] = saved
            cond = tuple((cid, flag_ap) for (cid, flag_ap, _) in self.cond_stack)
        else:
            waits = self._deps(eng, reads, writes)
        self.cnt[eng] = self.cnt.get(eng, 0) + 1
        ev = (eng, self.cnt[eng])
        self.ops[eng].append((waits, fn, eng, 1, cond, self.cnt[eng]))
        self._commit(ev, reads, writes)
        return ev

    def dma(self, q, sem, fn, reads=(), writes=(), final=False):
        waits = self._deps(q, reads, writes)
        self.cnt[sem] = self.cnt.get(sem, 0) + 16
        ev = (sem, self.cnt[sem])
        self.ops[q].append((waits, fn, sem, 16, None, self.cnt[sem]))
        self._commit(ev, reads, writes)
        if final:
            self.final.append(ev)
        return ev

    def emit(self):
        nc = self.nc
        keys = list(self.cnt.keys())
        with contextlib.ExitStack() as es:
            for k in keys:
                self.sems[k] = es.enter_context(nc.semaphore("s_" + str(k)))
            block = es.enter_context(nc.Block())
            sems = self.sems

            regs = {"pe": [es.enter_context(nc.tensor.register("rg_pe%d" % i)) for i in range(2)],
                    "act": [es.enter_context(nc.scalar.register("rg_act%d" % i)) for i in range(2)],
                    "dve": [es.enter_context(nc.vector.register("rg_dve%d" % i)) for i in range(2)]}

            def emit_one(e, item):
                waits, fn, semk, inc, _, _c = item
                for s, v in waits:
                    e.wait_ge(sems[s], v)
                fn(e).then_inc(sems[semk], inc)

            def emit_range(e, engname, ops, i, j, depth):
                k = i
                while k < j:
                    chain = ops[k][4] or ()
                    if len(chain) <= depth:
                        emit_one(e, ops[k])
                        k += 1
                        continue
                    cid, ap = chain[depth]
                    m = k
                    while m < j and ops[m][4] is not None and len(ops[m][4]) > depth and ops[m][4][depth][0] == cid:
                        m += 1
                    for s, v in self.cond_first[(cid, engname)]:
                        e.wait_ge(sems[s], v)
                    r = regs[engname][depth]
                    e.reg_load(r, ap)
                    with e.If_ne(r, 0):
                        emit_range(e, engname, ops, k, m, depth + 1)
                    with e.Else():
                        e.wait_ge(sems[engname], ops[k][5] - 1)
                        e.sem_inc(sems[engname], m - k)
                    k = m

            def replay(engname, e):
                ops = self.ops[engname]
                emit_range(e, engname, ops, 0, len(ops), 0)
                if engname == "sp":
                    fin = {}
                    for s, v in self.final:
                        fin[s] = max(fin.get(s, 0), v)
                    for s, v in fin.items():
                        e.wait_ge(sems[s], v)

            @block.sync
            def _(e):
                replay("sp", e)

            @block.tensor
            def _(e):
                replay("pe", e)

            @block.scalar
            def _(e):
                replay("act", e)

            @block.vector
            def _(e):
                replay("dve", e)

            @block.gpsimd
            def _(e):
                replay("pool", e)


def tokslice(p, b):
    if p == 0:
        return slice(b * 128, (b + 1) * 128)
    if p == 1:
        r, blk = b // 4, b % 4
        st = r + 512 * blk
        return slice(st, st + 4 * 127 + 1, 4)
    return slice(b, b + 16 * 127 + 1, 16)


def build(nseq=NSEQ, dbg=()):
    nc = bass.Bass("TRN2", target_bir_lowering=False)
    P = Prog(nc)

    def din(name, shape, dt=F32):
        return nc.dram_tensor(name, shape, dt, kind="ExternalInput").ap()
    x_d = din("x", [NSEQ, S, D]); c_d = din("c", [NSEQ, D])
    w_ada = din("w_ada", [D, 6 * D]); b_ada = din("b_ada", [6 * D]); n1g = din("norm1_g", [D])
    w_in = din("w_in", [D, 7 * D]); sgu_g = din("sgu_norm_g", [D]); sgu_wT = din("sgu_wT", [8, 128, 128])
    sgu_b = din("sgu_b", [8 * 128]); qg = din("q_norm_g", [128]); kg = din("k_norm_g", [128])
    w_pa = din("w_proj_a", [D, D]); w_pb = din("w_proj_b", [D, D]); w_out = din("w_out", [D, D])
    n2g = din("norm2_g", [D]); w_r = din("w_r", [D, 20]); b_r = din("b_r", [20])
    w_gate = din("w_gate", [N_EXP, D, 512]); w_up = din("w_up", [N_EXP, D, 512]); w_down = din("w_down", [N_EXP, 512, D])
    identb_d = din("identb", [128, 128], BF16); identf_d = din("identf", [128, 128])
    trilT_d = din("trilT", [128, 128]); amask_d = din("amask", [128, 3, 512], BF16)
    utri_d = din("utri", [128, 128], BF16); blkc_d = din("blkc", [128, 2, 8])
    y_d = nc.dram_tensor("y", [NSEQ, S, D], F32, kind="ExternalOutput").ap()
    X1_d = [nc.dram_tensor("scr_x1%d" % i, [S, D], F32, kind="Internal").ap() for i in range(NSEQ)]
    CW_d = [nc.dram_tensor("scr_cw%d" % i, [S, 17], F32, kind="Internal").ap() for i in range(NSEQ)]
    Y_d = [nc.dram_tensor("scr_y%d" % i, [S, D], F32, kind="Internal").ap() for i in range(NSEQ)]
    dbg_out = {}

    with contextlib.ExitStack() as es:
        def sb(name, shape, dt=F32):
            return es.enter_context(nc.sbuf_tensor("sb_" + name, shape, dt))

        def ps(name, shape, dt=F32):
            return es.enter_context(nc.psum_tensor("ps_" + name, shape, dt))
        R01 = sb("R01", [128, 32768], BF16)
        R2 = sb("R2", [128, 16384], BF16)
        R4 = sb("R4", [128, 14336], BF16)
        Wt = sb("Wt", [128, 6 * 4096], BF16)
        identb = sb("identb", [128, 128], BF16); identf = sb("identf", [128, 128])
        onesb = sb("onesb", [128, 128], BF16); amask = sb("amask", [128, 3, 512], BF16)
        vecs = sb("vecs", [80, 128]); colsT = sb("colsT", [128, 80]); condb = sb("condb", [128, 16], BF16)
        modc = sb("modc", [128, 48, 2]); Gc = sb("Gc", [128, 2, 2, 8])
        gaterow = sb("gaterow", [128, 2, 2, 1024])
        gsgu = sb("gsgu", [128, 1024]); WsT = sb("WsT", [128, 8, 128], BF16)
        bs2 = sb("bs2", [33, 1024], BF16); gqk = sb("gqk", [128, 2]); epsT = sb("epsT", [128, 1]); ones1 = sb("ones1", [128, 1])
        Wr = sb("Wr", [128, 8, 20], BF16); brow = sb("brow", [128, 20])
        logits = sb("logits", [128, 16, 20]); cw = sb("cw", [128, 16, 17])
        junk = sb("junk", [128, 1024], BF16)
        I32 = mybir.dt.int32
        utri = sb("utri", [128, 128], BF16); blkc = sb("blkc", [128, 2, 8]); ohb = sb("ohb", [128, 16, 4], BF16)
        tot = sb("tot", [128, 4]); stt = sb("stt", [128, 4]); ent = sb("ent", [128, 4]); cnt3 = sb("cnt3", [128, 16, 4])
        posf = sb("posf", [128, 16]); posi = sb("posi", [128, 16], I32)
        flA = sb("flA", [128, 4, 8]); flB = sb("flB", [128, 4, 8]); flags_i = sb("flags_i", [128, 32], I32)
        flQ = sb("flQ", [128, 4, 2]); flags_q = sb("flags_q", [128, 8], I32)
        ssA = sb("ssA", [128, 16]); lnA = sb("lnA", [128, 16]); rsA = sb("rsA", [128, 16])
        ssV = sb("ssV", [128, 4]); lnV = sb("lnV", [128, 4]); rsV = sb("rsV", [128, 4])
        psA = [ps("psA0", [128, 512]), ps("psA1", [128, 512])]
        psB = [ps("psB0", [128, 512]), ps("psB1", [128, 512])]
        psT = [ps("psT0", [128, 8, 128], BF16), ps("psT1", [128, 8, 128], BF16)]
        psS = ps("psS", [128, 512]); psO = ps("psO", [128, 512])

        def view(reg, off, shape, dt):
            n = int(np.prod(shape[1:]))
            nb = n * (4 if dt == F32 else 2)
            ap = reg[:, off // 2: (off + nb) // 2]
            if dt == F32:
                ap = ap.bitcast(F32)
            if len(shape) == 3:
                ap = ap.rearrange("p (a b) -> p a b", b=shape[2])
            elif len(shape) == 4:
                ap = ap.rearrange("p (a b c) -> p a b c", b=shape[2], c=shape[3])
            return ap
        rt = {}
        _o = 16384
        for n, sh in [("gmax", []), ("ohg", [4]), ("dg", [4]), ("eg", [4]), ("sumg", []), ("pg", []), ("tmp", [4, 4]),
                      ("sel", [4]), ("m1", []), ("oh1", [4]), ("sel2", [4]), ("m2", []), ("oh2", [4]), ("d", []),
                      ("ed", []), ("den", []), ("r", []), ("p1", []), ("p2", []), ("c1", [4]), ("c2", [4]), ("cwg", [4])]:
            shape = [128, 16] + sh
            rt[n] = view(R4, _o, shape, F32)
            _o += int(np.prod(shape[1:])) * 4
        RTN = ["rt_" + n for n in rt]
        K32 = 32768
        hT = view(R01, 0, [128, 8, S], BF16)
        oT = view(R01, K32, [128, 8, S], BF16)
        zT = oT
        acc = view(R01, 0, [128, 16, D], F32)
        mg = view(R2, 0, [128, 8, S], BF16)
        h2T = mg

        def wslot(i, shape):
            return view(Wt, i * 8192, shape, BF16)

        def wres(i, n=1):
            return [("W", (i + j) % 6) for j in range(n)]

        def mm(out, pairs, reads, writes):
            def fn(e, out=out, pairs=pairs):
                n = len(pairs)
                for i, (l, r) in enumerate(pairs):
                    ins = e.matmul(out, lhsT=l, rhs=r, start=(i == 0), stop=(i == n - 1))
                return ins
            P.op("pe", fn, reads, writes)

        def add_dbg(name, ap, shape, reads):
            if name in dbg:
                d = nc.dram_tensor("dbg_" + name, shape, ap.dtype, kind="ExternalOutput").ap()
                P.dma("sp", "dbg_" + name, lambda e, d=d, ap=ap: e.dma_start(out=d, in_=ap), reads=reads, final=True)

        cl = []

        def cdma(out, in_, res, q="sp"):
            P.dma(q, "const" if q == "sp" else "constp", lambda e, out=out, in_=in_: e.dma_start(out=out, in_=in_), writes=[res])
            if q == "sp":
                cl.append(res)
        cdma(identb[:], identb_d, "identb"); cdma(identf[:], identf_d, "identf"); cdma(amask[:], amask_d, "amask")
        cdma(vecs[0:8, :], n1g.rearrange("(k p) -> k p", p=128), "vecs")
        cdma(vecs[8:16, :], n2g.rearrange("(k p) -> k p", p=128), "vecs")
        cdma(vecs[16:64, :], b_ada.rearrange("(k p) -> k p", p=128), "vecs")
        cdma(vecs[64:80, :], c_d.rearrange("s (k p) -> (s k) p", p=128), "vecs")
        cdma(gqk[:, 0:1], qg.rearrange("(p o) -> p o", o=1), "gqk")
        cdma(gqk[:, 1:2], kg.rearrange("(p o) -> p o", o=1), "gqk")
        cdma(gsgu[:], sgu_g.partition_broadcast(128), "gsgu")
        cdma(brow[:], b_r.partition_broadcast(128), "brow")
        gbias = view(R2, 0, [128, 2, 1024], F32)
        cdma(gbias[:, 0, :], b_ada[2 * D:3 * D].partition_broadcast(128), "gbias")
        cdma(gbias[:, 1, :], b_ada[5 * D:6 * D].partition_broadcast(128), "gbias")
        wsf = view(R4, 8192, [128, 8, 128], F32)
        trilT = view(R4, 12288, [128, 128], F32)
        bsf = view(R4, 12800, [128, 1024], F32)
        bsh = view(R4, 16896, [128, 1024], F32)
        rep = view(R4, 20992, [128, 2, 8, 128], BF16)
        cdma(wsf, sgu_wT.rearrange("g s t -> s g t"), "wsf")
        cdma(trilT, trilT_d, "trilT")
        cdma(bsf[0:1, :], sgu_b.rearrange("(o n) -> o n", o=1), "bsf")
        cdma(bsf[32:33, :], sgu_b.rearrange("(o n) -> o n", o=1), "bsf")
        cdma(Wr[:], w_r.rearrange("(k p) n -> p k n", p=128), "Wr", q="pool")
        cdma(utri[:], utri_d, "utri"); cdma(blkc[:], blkc_d, "blkc")
        for r in set(cl):
            P.res[r][0]["const"] = P.cnt["const"]

        P.op("dve", lambda e: e.memset(onesb[:], 1.0), writes=["onesb"])
        P.op("dve", lambda e: e.memset(epsT[:], EPS), writes=["epsT"])
        P.op("dve", lambda e: e.memset(ones1[:], 1.0), writes=["ones1"])
        P.op("dve", lambda e: e.memset(bs2[:], 0.0), writes=["bs2"])
        P.op("dve", lambda e: e.tensor_tensor(out=WsT[:], in0=wsf, in1=trilT.unsqueeze(1).to_broadcast([128, 8, 128]), op=ALU.mult),
             reads=["wsf", "trilT"], writes=["WsT"])
        P.op("dve", lambda e: e.tensor_copy(out=bs2[0:1, :], in_=bsf[0:1, :]), reads=["bsf", "bs2"], writes=["bs2a"])
        P.op("dve", lambda e: e.tensor_copy(out=junk[32:33, :], in_=bsf[32:33, :]), reads=["bsf"], writes=["junk"])
        P.op("dve", lambda e: e.tensor_copy(out=bsh[32:33, :], in_=junk[32:33, :]), reads=["junk"], writes=["bsh"])
        P.op("dve", lambda e: e.tensor_tensor(out=bsh[32:33, :], in0=bsf[32:33, :], in1=bsh[32:33, :], op=ALU.subtract),
             reads=["bsh", "bsf"], writes=["bsh"])
        P.op("dve", lambda e: e.tensor_copy(out=bs2[32:33, :], in_=bsh[32:33, :]), reads=["bsh", "bs2"], writes=["bs2b"])
        BS2 = ["bs2a", "bs2b"]
        P.op("dve", lambda e: e.tensor_scalar(out=gqk[:, 0:1], in0=gqk[:, 0:1], scalar1=float(128 ** -0.5), scalar2=None, op0=ALU.mult),
             reads=["gqk"], writes=["gqk"])
        P.op("pe", lambda e: e.transpose(psA[0][:, 0:80], vecs[0:80, :], identf[0:80, 0:80]), reads=["vecs", "identf"], writes=["psA0"])
        P.op("dve", lambda e: e.tensor_copy(out=colsT[:], in_=psA[0][:, 0:80]), reads=["psA0"], writes=["colsT"])
        P.op("act", lambda e: e.activation(out=condb[:], in_=colsT[:, 64:80], func=AF.Silu), reads=["colsT"], writes=["condb"])
        for s in range(2):
            P.op("dve", lambda e, s=s: e.tensor_copy(out=rep[:, s, :, :], in_=condb[:, s * 8:(s + 1) * 8].unsqueeze(2).to_broadcast([128, 8, 128])),
                 reads=["condb"], writes=["rep"])
        def ada_load(j):
            sl = 2 * (j % 3)
            Wp = wslot(sl, [128, 8, 1024])
            P.dma("pool", "w%d" % sl, lambda e: e.dma_start(out=Wp, in_=w_ada[:, j * D:(j + 1) * D].rearrange("(k p) n -> p k n", p=128)),
                  writes=wres(sl, 2))
            return sl, Wp

        def ada_cols(slWp, bank, bname, c0):
            sl, Wp = slWp
            for m in range(8):
                mm(bank[:, (c0 + m) * 2:(c0 + m) * 2 + 2], [(Wp[:, k, m * 128:(m + 1) * 128], condb[:, k:16:8]) for k in range(8)],
                   reads=wres(sl, 2) + ["condb"], writes=[bname])

        def ada_rows(slWp, which):
            sl, Wp = slWp
            for s in range(2):
                for half in range(2):
                    mm(psA[half][:], [(rep[:, s, k, :], Wp[:, k, half * 512:(half + 1) * 512]) for k in range(8)],
                       reads=wres(sl, 2) + ["rep"], writes=["psA%d" % half])
                    P.op("dve", lambda e, s=s, half=half: e.tensor_tensor(
                        out=gaterow[:, s, which, half * 512:(half + 1) * 512], in0=psA[half][:],
                        in1=gbias[:, which, half * 512:(half + 1) * 512], op=ALU.add),
                        reads=["psA%d" % half, "gbias"], writes=["gaterow"])

        def ada_fin(wh, bank, bname):
            a0 = 24 * wh
            P.op("dve", lambda e: e.tensor_tensor(out=modc[:, a0:a0 + 16, :], in0=bank[:, 0:32].rearrange("p (a b) -> p a b", b=2),
                                                  in1=colsT[:, 16 + a0:32 + a0].unsqueeze(2).to_broadcast([128, 16, 2]), op=ALU.add),
                 reads=[bname, "colsT"], writes=[("modc", wh)])
            for s in range(2):
                P.op("dve", lambda e, s=s: e.scalar_tensor_tensor(
                    out=Gc[:, wh, s, :], in0=modc[:, a0 + 8:a0 + 16, s], scalar=1.0,
                    in1=colsT[:, wh * 8:(wh + 1) * 8], op0=ALU.add, op1=ALU.mult), reads=[("modc", wh), "colsT"], writes=[("Gc", wh)])
        adaL = {j: ada_load(j) for j in range(2)}
        ada_cols(adaL[0], psB[0], "psB0", 0)
        ada_cols(adaL[1], psB[0], "psB0", 8)
        ada_fin(0, psB[0], "psB0")
        for j in (2, 3, 4):
            adaL[j] = ada_load(j)

        def ada_part2():
            ada_rows(adaL[2], 0)
            ada_cols(adaL[3], psB[1], "psB1", 0)
            ada_cols(adaL[4], psB[1], "psB1", 8)
            adaL[5] = ada_load(5)
            ada_fin(1, psB[1], "psB1")
            ada_rows(adaL[5], 1)

        def shiftc(wh, s, k):
            return modc[:, 3 * wh * 8 + k, s:s + 1]

        R4A = ["xs0", "xs1", "xs2", "xnb0", "xnb1"]
        R4P = ["gbias", "wsf", "trilT", "bsf", "bsh", "rep"]
        R4B = ["accB", "lnt0", "lnt1", "rs0", "rs1"]
        R2B = ["qT", "kT", "vT", "vtok", "sq0", "sq1", "pT0", "pT1", "pT2"]
        R4C = ["gv0", "gv1", "gv2", "gv3", "vn", "ug0", "ug1"]
        R4D = ["heT0", "heT1", "sgD0", "sgD1", "stg0", "stg1"]
        xs = [view(R4, i * 4096, [128, 1024], F32) for i in range(3)]
        xnb = [view(R4, 12288 + i * 2048, [128, 1024], BF16) for i in range(2)]

        def rstd_small(ss_ap, ln_ap, rs_ap, scale, r, w):
            P.op("act", lambda e: e.activation(out=ln_ap, in_=ss_ap, func=AF.Ln, scale=scale, bias=epsT[:]), reads=r + ["epsT"], writes=[w + "_ln"])
            P.op("act", lambda e: e.activation(out=rs_ap, in_=ln_ap, func=AF.Exp, scale=-0.5), reads=[w + "_ln"], writes=[w])

        def norm_transpose(src_ap, src_res, tt, wh, s, dstT, dst_res, extra_w=(), defer=False):
            P.op("act", lambda e: e.activation(out=junk[:], in_=src_ap, func=AF.Square, accum_out=ssA[:, tt:tt + 1]),
                 reads=src_res, writes=["junk", ("ssA", tt)])
            rstd_small(ssA[:, tt:tt + 1], lnA[:, tt:tt + 1], rsA[:, tt:tt + 1], 1.0 / D, [("ssA", tt)], "rsA%d" % tt)
            xb = xnb[tt % 2]
            P.op("dve", lambda e: e.tensor_scalar(out=xb, in0=src_ap, scalar1=rsA[:, tt:tt + 1], scalar2=None, op0=ALU.mult),
                 reads=src_res + ["rsA%d" % tt], writes=["xnb%d" % (tt % 2)])
            if not defer:
                transpose_evac(tt, wh, s, dstT, dst_res, extra_w)

        def transpose_evac(tt, wh, s, dstT, dst_res, extra_w=()):
            xb = xnb[tt % 2]
            pt = psT[tt % 2]

            def tr(e):
                for k in range(8):
                    ins = e.transpose(pt[:, k, :], xb[:, k * 128:(k + 1) * 128], identb[:])
                return ins
            P.op("pe", tr, reads=["xnb%d" % (tt % 2), "identb"], writes=["psT%d" % (tt % 2)])

            def ev_act(e):
                for k in range(8):
                    ins = e.activation(out=dstT[:, k, tt * 128:(tt + 1) * 128], in_=pt[:, k, :], func=AF.Identity,
                                       scale=Gc[:, wh, s, k:k + 1], bias=shiftc(wh, s, k))
                return ins

            def ev_dve(e):
                for k in range(8):
                    ins = e.tensor_scalar(out=dstT[:, k, tt * 128:(tt + 1) * 128], in0=pt[:, k, :], scalar1=Gc[:, wh, s, k:k + 1],
                                          scalar2=shiftc(wh, s, k), op0=ALU.mult, op1=ALU.add)
                return ins
            if tt % 2 == 0:
                P.op("act", ev_act, reads=["psT%d" % (tt % 2), ("Gc", wh), ("modc", wh)], writes=[dst_res] + list(extra_w))
            else:
                P.op("dve", ev_dve, reads=["psT%d" % (tt % 2), ("Gc", wh), ("modc", wh)], writes=[dst_res] + list(extra_w))

        def seq_body(s, prevR4):
            P.alias(R4A, prevR4)
            P.alias([("hT", st) for st in range(4)], [("acc", tt, hh) for tt in range(16) for hh in range(2)])
            P.op("dve", lambda e: e.memset(ssA[:], 0.0), writes=[("ssA", i) for i in range(16)])
            for tt in range(16):
                sl = tt % 3
                P.dma("sp" if s == 0 else "act", "x%d" % sl, lambda e, sl=sl, tt=tt: e.dma_start(out=xs[sl], in_=x_d[s, tt * 128:(tt + 1) * 128, :]),
                      writes=["xs%d" % sl])
                norm_transpose(xs[sl], ["xs%d" % sl], tt, 0, s, hT, ("hT", tt // 4), defer=True)
                if tt >= 1:
                    transpose_evac(tt - 1, 0, s, hT, ("hT", (tt - 1) // 4))
            transpose_evac(15, 0, s, hT, ("hT", 3))
            add_dbg("hT%d" % s, hT, [128, 8, S], [("hT", st) for st in range(4)])
            if s == 0:
                ada_part2()

            P.alias(R4B, R4A + RTN + ["ot0", "ot1", "ot2", "rep"])
            P.alias(R2B, [("h2T", tt) for tt in range(16)] + R4D + ["gbias"])
            P.alias([("oT", h) for h in range(8)], [("acc", tt, hh) for tt in range(16) for hh in range(2)])
            qT = view(R2, 0, [128, S], BF16); kT = view(R2, 4096, [128, S], BF16); vT = view(R2, 8192, [128, S], BF16)
            vtok = view(R2, 12288, [128, 3, 16, 128], BF16)
            pT = [view(R2, 24576 + i * 1024, [128, 512], BF16) for i in range(3)]
            sqb = [view(R2, 27648 + i * 1024, [128, 512], BF16) for i in range(2)]
            accB = view(R4, 0, [128, 2, S], F32)
            lnt = [view(R4, 16384 + i * 2048, [128, 512], F32) for i in range(2)]
            rs = [view(R4, 20480 + i * 2048, [128, 512], F32) for i in range(2)]
            Sb = [(psS, "psS"), (psB[0], "psB0")]
            Ob = [(psO, "psO"), (psB[1], "psB1")]

            def load_head(h):
                sl = (s * 8 + h) % 6
                wv = wslot(sl, [128, 8, 3, 128])
                for j in range(3):
                    c0 = (2 + j) * D + h * 128
                    P.dma("pool", "w%d" % sl, lambda e, j=j, c0=c0: e.dma_start(
                        out=wv[:, :, j, :], in_=w_in[:, c0:c0 + 128].rearrange("(k p) n -> p k n", p=128)),
                        writes=wres(sl))
                return sl, wv
            def load16(slot, src):
                Wv = wslot(slot, [128, 8, 1024])
                P.dma("pool", "w%d" % slot, lambda e: e.dma_start(out=Wv, in_=src.rearrange("(k p) n -> p k n", p=128)), writes=wres(slot, 2))
                return Wv
            c4spec = {"Wgb": (0, w_in[:, 6 * D:7 * D]), "Wpb": (2, w_pb), "Wvs": (4, w_in[:, D:2 * D])}
            c4w = {}
            nxh = load_head(0)
            ci = 0
            for h in range(8):
                sl, wv = nxh
                if h + 1 < 8:
                    nxh = load_head(h + 1)
                if h == 6:
                    busy = {(s * 8 + 6) % 6, (s * 8 + 7) % 6}
                    for nm_, (slot_, src_) in c4spec.items():
                        if not ({slot_, slot_ + 1} & busy):
                            c4w[nm_] = load16(slot_, src_)
                items = [(j, st, dst, dn) for (j, dst, dn) in ((1, kT, "kT"), (2, vT, "vT"), (0, qT, "qT")) for st in range(4)]

                def post2(a, j, st, dst, dn):
                    tsl = slice(st * 512, (st + 1) * 512)
                    mm(psB[a][:], [(onesb[:], sqb[a])], reads=["onesb", "sq%d" % a], writes=["psB%d" % a])
                    P.op("act", lambda e: e.activation(out=lnt[a], in_=psB[a][:], func=AF.Ln, scale=1.0 / 128, bias=epsT[:]),
                         reads=["psB%d" % a, "epsT"], writes=["lnt%d" % a])
                    P.op("act", lambda e: e.activation(out=rs[a], in_=lnt[a], func=AF.Exp, scale=-0.5), reads=["lnt%d" % a], writes=["rs%d" % a])
                    P.op("dve", lambda e: e.scalar_tensor_tensor(out=dst[:, tsl], in0=dst[:, tsl], scalar=gqk[:, j:j + 1], in1=rs[a],
                                                                  op0=ALU.mult, op1=ALU.mult),
                         reads=[dn, "gqk", "rs%d" % a], writes=[dn])
                pend = None
                for (j, st, dst, dn) in items:
                    a = ci % 2
                    ci += 1
                    tsl = slice(st * 512, (st + 1) * 512)
                    mm(psA[a][:], [(wv[:, k, j, :], hT[:, k, tsl]) for k in range(8)],
                       reads=wres(sl) + [("hT", st)], writes=["psA%d" % a])
                    P.op("dve", lambda e, a=a, dst=dst, tsl=tsl: e.tensor_copy(out=dst[:, tsl], in_=psA[a][:]), reads=["psA%d" % a], writes=[dn])
                    if j != 2:
                        P.op("act", lambda e, a=a, dst=dst, tsl=tsl: e.activation(out=sqb[a], in_=dst[:, tsl], func=AF.Square), reads=[dn], writes=["sq%d" % a])
                    if pend is not None:
                        post2(*pend)
                    pend = (a, j, st, dst, dn) if j != 2 else None
                if pend is not None:
                    post2(*pend)
                if _STOP == 1:
                    continue
                for p in range(3):
                    for b0 in range(0, 16, 8):
                        t = (p * 2 + b0 // 8) % 2
                        pt = psT[t]

                        def trv(e, p=p, b0=b0, pt=pt):
                            for i in range(8):
                                ins = e.transpose(pt[:, i, :], vT[:, tokslice(p, b0 + i)], identb[:])
                            return ins
                        P.op("pe", trv, reads=["vT", "identb"], writes=["psT%d" % t])
                        P.op("dve", lambda e, p=p, b0=b0, pt=pt: e.tensor_copy(out=vtok[:, p, b0:b0 + 8, :], in_=pt[:, :, :]),
                             reads=["psT%d" % t], writes=["vtok"])
                if _STOP == 2:
                    continue
                units = [(p, qb0) for p in range(3) for qb0 in range(0, 16, 2)]

                def hasprev(p, qb):
                    return (p == 0 and qb > 0) or (p == 1 and qb % 4 > 0)

                def st_qk(u, p, qb0):
                    bank, bn = Sb[u % 2]
                    Sv = bank[:].rearrange("p (b h t) -> p b h t", b=2, h=2)

                    def fn(e):
                        for bl in range(2):
                            qb = qb0 + bl
                            tq = tokslice(p, qb)
                            ks = tokslice(p, qb - 1) if hasprev(p, qb) else tq
                            e.matmul(Sv[:, bl, 0, :], lhsT=kT[:, ks], rhs=qT[:, tq], start=True, stop=True)
                            ins = e.matmul(Sv[:, bl, 1, :], lhsT=kT[:, tq], rhs=qT[:, tq], start=True, stop=True)
                        return ins
                    P.op("pe", fn, reads=["kT", "qT"], writes=[bn])
                    pi = u % 3
                    mi = 2 if p == 2 else (1 if not hasprev(p, qb0) else 0)
                    P.op("act", lambda e: e.activation(out=pT[pi], in_=bank[:], func=AF.Exp), reads=[bn], writes=["pT%d" % pi])
                    P.op("pool", lambda e: e.tensor_tensor(out=pT[pi], in0=pT[pi], in1=amask[:, mi, :], op=ALU.mult),
                         reads=["pT%d" % pi, "amask"], writes=["pT%d" % pi])

                def st_pv(u, p, qb0):
                    bank, bn = Ob[u % 2]
                    Ov = bank[:].rearrange("p (b o t) -> p b o t", b=2, o=2)
                    pi = u % 3

                    def fn(e):
                        for bl in range(2):
                            qb = qb0 + bl
                            hp = hasprev(p, qb)
                            for o, lh in ((0, None), (1, onesb[:])):
                                if hp:
                                    e.matmul(Ov[:, bl, o, :], lhsT=(vtok[:, p, qb - 1, :] if o == 0 else lh), rhs=pT[pi][:, bl * 256:bl * 256 + 128],
                                             start=True, stop=False)
                                ins = e.matmul(Ov[:, bl, o, :], lhsT=(vtok[:, p, qb, :] if o == 0 else lh), rhs=pT[pi][:, bl * 256 + 128:bl * 256 + 256],
                                               start=(not hp), stop=True)
                        return ins
                    P.op("pe", fn, reads=["vtok", "onesb", "pT%d" % pi], writes=[bn])
                    if p == 0:
                        dv = accB[:, :, qb0 * 128:qb0 * 128 + 256].rearrange("p o (b t) -> p b o t", b=2)
                    elif p == 1:
                        st0 = (qb0 // 4) + 512 * (qb0 % 4)
                        dv = accB[:, :, st0:st0 + 1021:4].rearrange("p o (b t) -> p b o t", b=2)
                    else:
                        dv = accB[:, :, :].rearrange("p o (i r) -> p r o i", r=16)[:, qb0:qb0 + 2, :, :]
                    if p == 0:
                        P.op("dve", lambda e: e.tensor_copy(out=dv, in_=Ov), reads=[bn], writes=["accB"])
                    else:
                        P.op("dve", lambda e: e.tensor_tensor(out=dv, in0=Ov, in1=dv, op=ALU.add), reads=[bn, "accB"], writes=["accB"])
                pend_u = []
                for u, (p, qb0) in enumerate(units):
                    st_qk(u, p, qb0)
                    pend_u.append((u, p, qb0))
                    if len(pend_u) > 2:
                        st_pv(*pend_u.pop(0))
                while pend_u:
                    st_pv(*pend_u.pop(0))
                for st in range(4):
                    a = st % 2
                    P.op("act", lambda e, a=a, st=st: e.activation(out=lnt[a], in_=accB[:, 1, st * 512:(st + 1) * 512], func=AF.Ln),
                         reads=["accB"], writes=["lnt%d" % a])
                    P.op("act", lambda e, a=a: e.activation(out=rs[a], in_=lnt[a], func=AF.Exp, scale=-1.0), reads=["lnt%d" % a], writes=["rs%d" % a])
                    P.op("dve", lambda e, a=a, st=st, h=h: e.tensor_tensor(out=oT[:, h, st * 512:(st + 1) * 512], in0=accB[:, 0, st * 512:(st + 1) * 512],
                                                                         in1=rs[a], op=ALU.mult),
                         reads=["accB", "rs%d" % a], writes=[("oT", h)])
            add_dbg("oT%d" % s, oT, [128, 8, S], [("oT", h) for h in range(8)])
            if _STOP:
                return R4B

            P.alias(R4C + ["sg0", "sg1", "tmpc0", "tmpc1"], R4B)
            P.alias([("mg", st) for st in range(4)], R2B)
            sg = [view(R4, 24576 + i * 2048, [128, 512], F32) for i in range(2)]
            gv = [view(R4, i * 4096, [128, 1024], F32) for i in range(4)]
            vn = view(R4, 16384, [128, 4, 1024], BF16)
            ug = sg

            for nm_ in ("Wgb", "Wpb", "Wvs"):
                if nm_ not in c4w:
                    c4w[nm_] = load16(*c4spec[nm_])
            Wgb, Wpb, Wvs = c4w["Wgb"], c4w["Wpb"], c4w["Wvs"]
            ci = 0
            for st in range(4):
                tsl = slice(st * 512, (st + 1) * 512)
                for ct in range(8):
                    a = ci % 2
                    ci += 1
                    csl = slice(ct * 128, (ct + 1) * 128)
                    mm(psA[a][:], [(Wgb[:, k, csl], hT[:, k, tsl]) for k in range(8)], reads=wres(0, 2) + [("hT", st)], writes=["psA%d" % a])
                    mm(psB[a][:], [(Wpb[:, k, csl], oT[:, k, tsl]) for k in range(8)], reads=wres(2, 2) + [("oT", k) for k in range(8)], writes=["psB%d" % a])
                    P.op("act", lambda e, a=a: e.activation(out=sg[a], in_=psA[a][:], func=AF.Sigmoid), reads=["psA%d" % a], writes=["sg%d" % a])
                    P.op("dve", lambda e, a=a, ct=ct, tsl=tsl: e.tensor_tensor(out=mg[:, ct, tsl], in0=psB[a][:], in1=sg[a], op=ALU.mult),
                         reads=["psB%d" % a, "sg%d" % a], writes=[("mg", st)])
            P.alias([("zT", st) for st in range(4)], [("oT", h) for h in range(8)])
            Wu = load16(0, w_in[:, 0:D])
            Wga = load16(2, w_in[:, 5 * D:6 * D])
            for st in range(4):
                tsl = slice(st * 512, (st + 1) * 512)
                P.op("dve", lambda e: e.memset(ssV[:], 0.0), writes=[("ssV", i) for i in range(4)])
                for t4 in range(4):
                    tt = st * 4 + t4
                    for half in range(2):
                        mm(psA[half][:], [(hT[:, k, tt * 128:(tt + 1) * 128], Wvs[:, k, half * 512:(half + 1) * 512]) for k in range(8)],
                           reads=wres(4, 2) + [("hT", st)], writes=["psA%d" % half])
                        P.op("act", lambda e, half=half, t4=t4: e.activation(out=gv[t4][:, half * 512:(half + 1) * 512], in_=psA[half][:], func=AF.Gelu_apprx_tanh),
                             reads=["psA%d" % half], writes=["gv%d" % t4])
                    P.op("act", lambda e, t4=t4: e.activation(out=junk[:], in_=gv[t4], func=AF.Square, accum_out=ssV[:, t4:t4 + 1]),
                         reads=["gv%d" % t4], writes=["junk", ("ssV", t4)])
                rstd_small(ssV[:], lnV[:], rsV[:], 1.0 / D, [("ssV", i) for i in range(4)], "rsV")
                for t4 in range(4):
                    P.op("dve", lambda e, t4=t4: e.scalar_tensor_tensor(out=vn[:, t4, :], in0=gv[t4], scalar=rsV[:, t4:t4 + 1], in1=gsgu[:], op0=ALU.mult, op1=ALU.mult),
                         reads=["gv%d" % t4, "rsV", "gsgu"], writes=["vn"])
                for g in range(8):
                    a = g % 2
                    csl = slice(g * 128, (g + 1) * 128)
                    mm(psB[a][:], [(Wu[:, k, csl], hT[:, k, tsl]) for k in range(8)], reads=wres(0, 2) + [("hT", st)], writes=["psB%d" % a])
                    P.op("act", lambda e, a=a: e.activation(out=ug[a], in_=psB[a][:], func=AF.Gelu_apprx_tanh), reads=["psB%d" % a], writes=["sg%d" % a])
                    pm = psS if a == 0 else psO
                    pmn = "psS" if a == 0 else "psO"
                    pmn2 = pmn

                    def mix(e, g=g, pm=pm, csl=csl):
                        for t4 in range(4):
                            e.matmul(pm[:, t4 * 128:(t4 + 1) * 128], lhsT=vn[:, t4, csl], rhs=WsT[:, g, :], start=True, stop=False)
                            ins = e.matmul(pm[:, t4 * 128:(t4 + 1) * 128], lhsT=onesb[0:33, :], rhs=bs2[0:33, csl], start=False, stop=True)
                        return ins
                    P.op("pe", mix, reads=["vn", "WsT", "onesb"] + BS2, writes=[pmn, pmn2])
                    P.op("dve", lambda e, a=a, g=g, pm=pm, tsl=tsl: e.tensor_tensor(out=zT[:, g, tsl], in0=pm[:], in1=ug[a], op=ALU.mult),
                         reads=[pmn, pmn2, "sg%d" % a], writes=[("zT", st)])
            add_dbg("zT%d" % s, zT, [128, 8, S], [("zT", st) for st in range(4)])
            Wpa = load16(4, w_pa)
            Wo = load16(0, w_out)
            tmpc = [view(R4, i * 2048, [128, 512], F32) for i in range(2)]
            P.alias(["tmpc0", "tmpc1"], ["gv0"])
            ci = 0
            for st in range(4):
                tsl = slice(st * 512, (st + 1) * 512)
                for ct in range(8):
                    a = ci % 2
                    ci += 1
                    csl = slice(ct * 128, (ct + 1) * 128)
                    mm(psA[a][:], [(Wga[:, k, csl], hT[:, k, tsl]) for k in range(8)], reads=wres(2, 2) + [("hT", st)], writes=["psA%d" % a])
                    mm(psB[a][:], [(Wpa[:, k, csl], zT[:, k, tsl]) for k in range(8)], reads=wres(4, 2) + [("zT", st)], writes=["psB%d" % a])
                    P.op("act", lambda e, a=a: e.activation(out=sg[a], in_=psA[a][:], func=AF.Sigmoid), reads=["psA%d" % a], writes=["sg%d" % a])
                    P.op("dve", lambda e, a=a: e.tensor_tensor(out=tmpc[a], in0=psB[a][:], in1=sg[a], op=ALU.mult),
                         reads=["psB%d" % a, "sg%d" % a], writes=["tmpc%d" % a])
                    P.op("pool", lambda e, a=a, ct=ct, tsl=tsl: e.tensor_tensor(out=mg[:, ct, tsl], in0=tmpc[a], in1=mg[:, ct, tsl], op=ALU.add),
                         reads=["tmpc%d" % a, ("mg", st)], writes=[("mg", st)])
            add_dbg("mg%d" % s, mg, [128, 8, S], [("mg", st) for st in range(4)])
            R4E = ["xs0", "xs1", "xs2", "xnb0", "xnb1"]
            P.alias(R4E + RTN, R4C + ["sg0", "sg1", "tmpc0", "tmpc1"])
            P.alias([("acc", tt, hh) for tt in range(16) for hh in range(2)], [("hT", st) for st in range(4)] + [("zT", st) for st in range(4)])
            P.op("dve", lambda e: e.memset(ssA[:], 0.0), writes=[("ssA", i) for i in range(16)])
            P.op("dve", lambda e: e.tensor_tensor(out=Wo, in0=Wo, in1=gaterow[:, s, 0, :].unsqueeze(1).to_broadcast([128, 8, 1024]), op=ALU.mult),
                 reads=wres(0, 2) + ["gaterow"], writes=wres(0, 2))
            for tt in range(16):
                sl = tt % 3
                P.dma("sp", "x%d" % sl, lambda e, sl=sl, tt=tt: e.dma_start(out=xs[sl], in_=x_d[s, tt * 128:(tt + 1) * 128, :]),
                      writes=["xs%d" % sl])
                for half in range(2):
                    hs = slice(half * 512, (half + 1) * 512)
                    mm(psA[half][:], [(mg[:, k, tt * 128:(tt + 1) * 128], Wo[:, k, hs]) for k in range(8)],
                       reads=wres(0, 2) + [("mg", tt // 4), ("mgt", tt)], writes=["psA%d" % half])
                    P.op("dve", lambda e, half=half, hs=hs, tt=tt, sl=sl: e.tensor_tensor(out=acc[:, tt, hs], in0=psA[half][:], in1=xs[sl][:, hs], op=ALU.add),
                         reads=["psA%d" % half, "xs%d" % sl], writes=[("acc", tt, half)])
                norm_transpose(acc[:, tt, :], [("acc", tt, 0), ("acc", tt, 1)], tt, 1, s, h2T, ("h2T", tt), extra_w=[("mgt", tt)], defer=True)

                def c5_stage2(tt):
                    transpose_evac(tt, 1, s, h2T, ("h2T", tt), [("mgt", tt)])
                if tt >= 1:
                    c5_stage2(tt - 1)
            c5_stage2(15)
            lgv = psS[:, 0:320].rearrange("p (t n) -> p t n", n=20)

            def lg(e):
                for tt in range(16):
                    for k in range(8):
                        ins = e.matmul(lgv[:, tt, :], lhsT=h2T[:, k, tt * 128:(tt + 1) * 128], rhs=Wr[:, k, :], start=(k == 0), stop=(k == 7))
                return ins
            P.op("pe", lg, reads=[("h2T", tt) for tt in range(16)] + ["Wr"], writes=["psS"])
            P.op("dve", lambda e: e.tensor_tensor(out=logits[:], in0=lgv, in1=brow[:].unsqueeze(1).to_broadcast([128, 16, 20]), op=ALU.add),
                 reads=["psS", "brow"], writes=["logits"])
            add_dbg("x1_%d" % s, acc, [128, 16, D], [("acc", tt, hh) for tt in range(16) for hh in range(2)])
            add_dbg("h2T%d" % s, h2T, [128, 8, S], [("h2T", tt) for tt in range(16)])
            add_dbg("logits%d" % s, logits[:], [128, 16, 20], ["logits"])
            Lg = logits[:, :, 0:4]
            Le = logits[:, :, 4:20].rearrange("p t (g e) -> p t g e", e=4)

            def bc3(ap):
                return ap.unsqueeze(2).to_broadcast([128, 16, 4])

            def dv(fn, r, w):
                P.op("dve", fn, reads=["rt_" + n if n in rt else n for n in r], writes=["rt_" + n if n in rt else n for n in w])
            R = rt
            dv(lambda e: e.tensor_reduce(out=R["gmax"], in_=Lg, axis=AX.X, op=ALU.max), ["logits"], ["gmax"])
            dv(lambda e: e.tensor_tensor(out=R["ohg"], in0=Lg, in1=bc3(R["gmax"]), op=ALU.is_equal), ["logits", "gmax"], ["ohg"])
            dv(lambda e: e.tensor_copy(out=R["m1"], in_=R["ohg"][:, :, 0]), ["ohg"], ["m1"])
            for g_ in range(1, 4):
                dv(lambda e: e.tensor_scalar(out=R["m2"], in0=R["m1"], scalar1=-1.0, scalar2=1.0, op0=ALU.mult, op1=ALU.add), ["m1"], ["m2"])
                dv(lambda e, g_=g_: e.tensor_tensor(out=R["ohg"][:, :, g_], in0=R["ohg"][:, :, g_], in1=R["m2"], op=ALU.mult), ["ohg", "m2"], ["ohg"])
                if g_ < 3:
                    dv(lambda e, g_=g_: e.tensor_tensor(out=R["m1"], in0=R["m1"], in1=R["ohg"][:, :, g_], op=ALU.max), ["m1", "ohg"], ["m1"])
            dv(lambda e: e.tensor_tensor(out=R["dg"], in0=Lg, in1=bc3(R["gmax"]), op=ALU.subtract), ["logits", "gmax"], ["dg"])
            P.op("act", lambda e: e.activation(out=R["eg"], in_=R["dg"], func=AF.Exp), reads=["rt_dg"], writes=["rt_eg"])
            dv(lambda e: e.tensor_reduce(out=R["sumg"], in_=R["eg"], axis=AX.X, op=ALU.add), ["eg"], ["sumg"])
            dv(lambda e: e.reciprocal(out=R["pg"], in_=R["sumg"]), ["sumg"], ["pg"])
            dv(lambda e: e.tensor_tensor(out=R["tmp"], in0=Le, in1=R["ohg"].unsqueeze(3).to_broadcast([128, 16, 4, 4]), op=ALU.mult), ["logits", "ohg"], ["tmp"])
            dv(lambda e: e.tensor_reduce(out=R["sel"], in_=R["tmp"].rearrange("p t g e -> p t e g"), axis=AX.X, op=ALU.add), ["tmp"], ["sel"])
            dv(lambda e: e.tensor_reduce(out=R["m1"], in_=R["sel"], axis=AX.X, op=ALU.max), ["sel"], ["m1"])
            dv(lambda e: e.tensor_tensor(out=R["oh1"], in0=R["sel"], in1=bc3(R["m1"]), op=ALU.is_equal), ["sel", "m1"], ["oh1"])
            dv(lambda e: e.scalar_tensor_tensor(out=R["sel2"], in0=R["oh1"], scalar=-1e30, in1=R["sel"], op0=ALU.mult, op1=ALU.add), ["oh1", "sel"], ["sel2"])
            dv(lambda e: e.tensor_reduce(out=R["m2"], in_=R["sel2"], axis=AX.X, op=ALU.max), ["sel2"], ["m2"])
            dv(lambda e: e.tensor_tensor(out=R["oh2"], in0=R["sel2"], in1=bc3(R["m2"]), op=ALU.is_equal), ["sel2", "m2"], ["oh2"])
            dv(lambda e: e.tensor_tensor(out=R["d"], in0=R["m2"], in1=R["m1"], op=ALU.subtract), ["m2", "m1"], ["d"])
            P.op("act", lambda e: e.activation(out=R["ed"], in_=R["d"], func=AF.Exp), reads=["rt_d"], writes=["rt_ed"])
            dv(lambda e: e.tensor_scalar(out=R["den"], in0=R["ed"], scalar1=1.0, scalar2=None, op0=ALU.add), ["ed"], ["den"])
            dv(lambda e: e.reciprocal(out=R["r"], in_=R["den"]), ["den"], ["r"])
            dv(lambda e: e.tensor_tensor(out=R["p1"], in0=R["r"], in1=R["pg"], op=ALU.mult), ["r", "pg"], ["p1"])
            dv(lambda e: e.tensor_tensor(out=R["p2"], in0=R["ed"], in1=R["p1"], op=ALU.mult), ["ed", "p1"], ["p2"])
            dv(lambda e: e.tensor_tensor(out=R["c1"], in0=R["oh1"], in1=bc3(R["p1"]), op=ALU.mult), ["oh1", "p1"], ["c1"])
            dv(lambda e: e.tensor_tensor(out=R["c2"], in0=R["oh2"], in1=bc3(R["p2"]), op=ALU.mult), ["oh2", "p2"], ["c2"])
            dv(lambda e: e.tensor_tensor(out=R["cwg"], in0=R["c1"], in1=R["c2"], op=ALU.add), ["c1", "c2"], ["cwg"])
            cw4 = cw[:, :, 0:16].rearrange("p t (g e) -> p t g e", e=4)
            dv(lambda e: e.tensor_tensor(out=cw4, in0=R["ohg"].unsqueeze(3).to_broadcast([128, 16, 4, 4]),
                                         in1=R["cwg"].unsqueeze(2).to_broadcast([128, 16, 4, 4]), op=ALU.mult), ["ohg", "cwg"], ["cw"])
            dv(lambda e: e.tensor_copy(out=cw[:, :, 16], in_=rsA[:, 0:16]), ["rsA%d" % i for i in range(16)] + ["cw"], ["cw"])
            add_dbg("cw%d" % s, cw[:, :, 0:16], [128, 16, 16], ["cw"])

            dv(lambda e: e.tensor_copy(out=ohb[:], in_=R["ohg"]), ["ohg"], ["ohb"])
            cntv = psS[:, 0:64].rearrange("p (t g) -> p t g", g=4)

            def cums(e):
                for i in range(16):
                    for j in range(i):
                        e.matmul(cntv[:, i, :], lhsT=onesb[:], rhs=ohb[:, j, :], start=(j == 0), stop=False)
                    ins = e.matmul(cntv[:, i, :], lhsT=utri[:], rhs=ohb[:, i, :], start=(i == 0), stop=True)
                return ins
            P.op("pe", cums, reads=["ohb", "onesb", "utri"], writes=["psS"])
            mm(psO[:, 0:4], [(onesb[:], ohb[:, j, :]) for j in range(16)], reads=["ohb", "onesb"], writes=["psO"])
            dv(lambda e: e.tensor_copy(out=tot[:], in_=psO[:, 0:4]), ["psO"], ["tot"])
            dv(lambda e: e.tensor_copy(out=cnt3[:], in_=cntv), ["psS"], ["cnt3"])
            dv(lambda e: e.memset(stt[:], -1.0), [], ["stt"])
            for g in range(1, 4):
                dv(lambda e, g=g: e.tensor_tensor(out=stt[:, g:g + 1], in0=stt[:, g - 1:g], in1=tot[:, g - 1:g], op=ALU.add), ["stt", "tot"], ["stt"])
            dv(lambda e: e.tensor_tensor(out=ent[:], in0=stt[:], in1=tot[:], op=ALU.add), ["stt", "tot"], ["ent"])
            dv(lambda e: e.tensor_tensor(out=cnt3[:], in0=cnt3[:], in1=stt[:].unsqueeze(1).to_broadcast([128, 16, 4]), op=ALU.add), ["cnt3", "stt"], ["cnt3"])
            dv(lambda e: e.tensor_tensor(out=cnt3[:], in0=cnt3[:], in1=R["ohg"], op=ALU.mult), ["cnt3", "ohg"], ["cnt3"])
            dv(lambda e: e.tensor_reduce(out=posf[:], in_=cnt3[:], axis=AX.X, op=ALU.add), ["cnt3"], ["posf"])
            dv(lambda e: e.tensor_copy(out=posi[:], in_=posf[:]), ["posf"], ["posi"])
            dv(lambda e: e.tensor_tensor(out=flA[:], in0=stt[:].unsqueeze(2).to_broadcast([128, 4, 8]),
                                         in1=blkc[:, 0, :].unsqueeze(1).to_broadcast([128, 4, 8]), op=ALU.is_lt), ["stt", "blkc"], ["flA"])
            dv(lambda e: e.tensor_tensor(out=flB[:], in0=ent[:].unsqueeze(2).to_broadcast([128, 4, 8]),
                                         in1=blkc[:, 1, :].unsqueeze(1).to_broadcast([128, 4, 8]), op=ALU.is_ge), ["ent", "blkc"], ["flB"])
            dv(lambda e: e.tensor_tensor(out=flA[:], in0=flA[:], in1=flB[:], op=ALU.mult), ["flA", "flB"], ["flA"])
            dv(lambda e: e.tensor_copy(out=flags_i[:], in_=flA[:].rearrange("p g b -> p (g b)")), ["flA"], ["flags"])
            dv(lambda e: e.tensor_reduce(out=flQ[:], in_=flA[:].rearrange("p g (q b) -> p g q b", b=4), axis=AX.X, op=ALU.max), ["flA"], ["flQ"])
            dv(lambda e: e.tensor_copy(out=flags_q[:], in_=flQ[:].rearrange("p g q -> p (g q)")), ["flQ"], ["flags"])
            add_dbg("posf%d" % s, posf[:], [128, 16], ["posf"])
            add_dbg("flA%d" % s, flA[:], [128, 4, 8], ["flA"])
            for tt in range(16):
                idx = bass.IndirectOffsetOnAxis(ap=posi[:, tt:tt + 1], axis=0)
                P.dma("pool", "sc1", lambda e, tt=tt, idx=idx: e.indirect_dma_start(out=X1_d[s], out_offset=idx, in_=acc[:, tt, :], in_offset=None),
                      reads=[("acc", tt, 0), ("acc", tt, 1), "posi"], writes=["X1"])
                P.dma("pool", "scc", lambda e, tt=tt, idx=idx: e.indirect_dma_start(out=CW_d[s], out_offset=idx, in_=cw[:, tt, :], in_offset=None),
                      reads=["cw", "posi"], writes=["CW"])
            P.dma("sp", "rbc", lambda e: e.dma_start(out=cw[:], in_=CW_d[s].rearrange("(t p) e -> p t e", p=128)), reads=["CW"], writes=["cw"])
            for j in range(16):
                P.dma("sp", "rb%d" % j, lambda e, j=j: e.dma_start(out=acc[:, j, :], in_=X1_d[s][j * 128:(j + 1) * 128, :]),
                      reads=["X1"], writes=[("acc", j, 0), ("acc", j, 1)])
            for j in range(16):
                P.op("dve", lambda e, j=j: e.tensor_scalar(out=xnb[j % 2], in0=acc[:, j, :], scalar1=cw[:, j, 16:17], scalar2=None, op0=ALU.mult),
                     reads=[("acc", j, 0), ("acc", j, 1), "cw"], writes=["xnb%d" % (j % 2)])
                transpose_evac(j, 1, s, h2T, ("h2T", j))
            P.alias(R4D, R4E)
            heT = [view(R4, i * 4096, [128, 4, 512], BF16) for i in range(2)]
            sgD = [view(R4, 8192 + i * 2048, [128, 512], F32) for i in range(2)]
            psY = [psA[0], psB[0], psA[1], psB[1], psS, psO, psS, psO]
            psYn = [["psA0"], ["psB0"], ["psA1"], ["psB1"], ["psS"], ["psO"], ["psS"], ["psO"]]

            stg = [view(R4, 12288 + i * 8192, [128, 2048], F32) for i in range(2)]
            stgc = [0]

            def load_expert(e_):
                b = (3 * e_) % 6
                Wg = wslot(b, [128, 8, 512]); Wu_ = wslot((b + 1) % 6, [128, 8, 512]); Wd = wslot((b + 2) % 6, [128, 4, 1024])
                dmas, casts = [], []
                for mi, (Wdst, src) in enumerate(((Wg, w_gate), (Wu_, w_up), (Wd, w_down))):
                    for hh in range(2):
                        si = stgc[0] % 2
                        stgc[0] += 1
                        sres = "stg%d" % si
                        if mi < 2:
                            sv = stg[si].rearrange("p (k n) -> p k n", n=512)
                            srcap = src[e_, hh * 512:(hh + 1) * 512, :].rearrange("(k p) n -> p k n", p=128)
                            dstap = Wdst[:, hh * 4:(hh + 1) * 4, :]
                        else:
                            sv = stg[si].rearrange("p (k n) -> p k n", n=1024)
                            srcap = src[e_, hh * 256:(hh + 1) * 256, :].rearrange("(k p) n -> p k n", p=128)
                            dstap = Wdst[:, hh * 2:(hh + 1) * 2, :]
                        dmas.append(lambda sres=sres, sv=sv, srcap=srcap: P.dma(
                            "sp", sres, lambda e: e.dma_start(out=sv, in_=srcap), writes=[sres]))
                        if mi < 2:
                            casts.append(lambda sres=sres, sv=sv, dstap=dstap, mi=mi: P.op(
                                "pool", lambda e: e.tensor_tensor(out=dstap, in0=sv, in1=ones1[:, 0:1].unsqueeze(2).to_broadcast([128, 4, 512]), op=ALU.mult),
                                reads=[sres, "ones1"], writes=wres(b + mi)))
                        else:
                            casts.append(lambda sres=sres, sv=sv, dstap=dstap, mi=mi: P.op("pool", lambda e: e.tensor_tensor(
                                out=dstap, in0=sv, in1=gaterow[:, s, 1, :].unsqueeze(1).to_broadcast([128, 2, 1024]), op=ALU.mult),
                                reads=[sres, "gaterow"], writes=wres(b + mi)))
                d, c = dmas, casts
                steps = [d[0], d[1], c[0], d[2], c[1], d[3], c[2], d[4], c[3], d[5], c[4], c[5]]
                return (Wg, Wu_, Wd, b), steps
            nxt, steps0 = load_expert(0)
            for st_ in steps0:
                st_()
            ci = 0
            yi = 0
            hbi = 0
            for e_ in range(N_EXP):
                Wg, Wu_, Wd, b = nxt
                rest = []
                if e_ + 1 < N_EXP:
                    nxt, rest = load_expert(e_ + 1)
                    rest = list(rest)
                    while rest:
                        rest.pop(0)()
                sched = [2, 1, 1, 1, 1, 1, 1, 2]
                g = e_ // 4
                for blk in range(4):
                    P.cond_begin(flags_i[0:1, g * 8 + blk:g * 8 + blk + 1], "flags")
                    tsl = slice(blk * 512, (blk + 1) * 512)
                    hb = hbi % 2
                    hbi += 1
                    h2r = [("h2T", blk * 4 + i) for i in range(4)]
                    for f in range(4):
                        a = ci % 2
                        ci += 1
                        fsl = slice(f * 128, (f + 1) * 128)
                        mm(psA[a][:], [(Wg[:, k, fsl], h2T[:, k, tsl]) for k in range(8)], reads=wres(b) + h2r, writes=["psA%d" % a])
                        mm(psB[a][:], [(Wu_[:, k, fsl], h2T[:, k, tsl]) for k in range(8)], reads=wres(b + 1) + h2r, writes=["psB%d" % a])
                        P.op("act", lambda e, a=a: e.activation(out=sgD[a], in_=psA[a][:], func=AF.Silu), reads=["psA%d" % a], writes=["sgD%d" % a])
                        P.op("dve", lambda e, a=a, hb=hb, f=f: e.tensor_tensor(out=heT[hb][:, f, :], in0=psB[a][:], in1=sgD[a], op=ALU.mult),
                             reads=["psB%d" % a, "sgD%d" % a], writes=["heT%d" % hb])
                    for t4 in range(4):
                        tt = blk * 4 + t4
                        for half in range(2):
                            y = t4 * 2 + half
                            hs = slice(half * 512, (half + 1) * 512)
                            mm(psY[y][:], [(heT[hb][:, f, t4 * 128:(t4 + 1) * 128], Wd[:, f, hs]) for f in range(4)],
                               reads=wres(b + 2) + ["heT%d" % hb], writes=psYn[y])
                            P.op("dve", lambda e, y=y, tt=tt, hs=hs, e_=e_: e.scalar_tensor_tensor(
                                out=acc[:, tt, hs], in0=psY[y][:], scalar=cw[:, tt, e_:e_ + 1], in1=acc[:, tt, hs], op0=ALU.mult, op1=ALU.add),
                                reads=psYn[y] + ["cw", ("acc", tt, half)], writes=[("acc", tt, half)])
                    P.cond_end()
            ot = [view(R4, 16384 + i * 4096, [128, 1024], F32) for i in range(3)]
            OTN = ["ot0", "ot1", "ot2"]
            P.alias(OTN, R4D + R4E + RTN)
            for j in range(16):
                P.dma("sp", "sy", lambda e, j=j: e.dma_start(out=Y_d[s][j * 128:(j + 1) * 128, :], in_=acc[:, j, :]), reads=[("acc", j, 0), ("acc", j, 1)], writes=["Y"])
            for tt in range(16):
                o = tt % 3
                idx = bass.IndirectOffsetOnAxis(ap=posi[:, tt:tt + 1], axis=0)
                P.dma("pool", "gy%d" % o, lambda e, o=o, idx=idx: e.indirect_dma_start(out=ot[o], out_offset=None, in_=Y_d[s], in_offset=idx),
                      reads=["Y", "posi"], writes=[OTN[o]])
                P.dma("sp", "st%d" % tt, lambda e, tt=tt, o=o: e.dma_start(out=y_d[s, tt * 128:(tt + 1) * 128, :], in_=ot[o]),
                      reads=[OTN[o]], final=True)
            return R4D + R4E
        prevR4 = R4P
        for s_ in range(nseq):
            prevR4 = seq_body(s_, prevR4)
        P.emit()
    return nc


_CACHE = {}


def _consts():
    tril = np.tril(np.ones((128, 128), np.float32))
    trilT = np.ascontiguousarray(tril.T)
    tk = np.arange(128)[:, None]
    tq = np.arange(128)[None, :]
    pm = (tk >= tq).astype(np.float32); cm = (tk <= tq).astype(np.float32); z = np.zeros_like(pm)
    amask = np.stack([np.concatenate([pm, cm, pm, cm], 1), np.concatenate([z, cm, pm, cm], 1), np.concatenate([z, cm, z, cm], 1)], 1)
    amask = np.ascontiguousarray(amask).astype(ml_dtypes.bfloat16)
    blk = np.arange(8, dtype=np.float32) * 512
    blkc = np.ascontiguousarray(np.broadcast_to(np.stack([blk + 511, blk], 0)[None], (128, 2, 8))).astype(np.float32)
    return {
        "utri": np.triu(np.ones((128, 128), np.float32)).astype(ml_dtypes.bfloat16),
        "blkc": blkc,
        "identb": np.eye(128, dtype=np.float32).astype(ml_dtypes.bfloat16),
        "identf": np.eye(128, dtype=np.float32),
        "trilT": trilT,
        "amask": amask,
    }


def make_in_maps(inp, n_cores=8):
    f = lambda a: np.ascontiguousarray(np.asarray(a, dtype=np.float32))
    w_r = np.concatenate([f(inp["w_router_group"])[0]] + [f(inp["w_router_expert"])[0, g] for g in range(4)], axis=1)
    b_r = np.concatenate([f(inp["b_router_group"])[0].reshape(-1), f(inp["b_router_expert"])[0].reshape(-1)])
    shared = {
        "w_ada": f(inp["w_ada"])[0], "b_ada": f(inp["b_ada"])[0], "norm1_g": f(inp["norm1_g"])[0],
        "w_in": f(inp["w_in"])[0], "sgu_norm_g": f(inp["sgu_norm_g"])[0],
        "sgu_wT": np.ascontiguousarray(f(inp["sgu_w"])[0].transpose(0, 2, 1)),
        "sgu_b": f(inp["sgu_b"])[0].reshape(-1), "q_norm_g": f(inp["q_norm_g"])[0], "k_norm_g": f(inp["k_norm_g"])[0],
        "w_proj_a": f(inp["w_proj_a"])[0], "w_proj_b": f(inp["w_proj_b"])[0], "w_out": f(inp["w_out"])[0],
        "norm2_g": f(inp["norm2_g"])[0], "w_r": np.ascontiguousarray(w_r), "b_r": np.ascontiguousarray(b_r),
        "w_gate": f(inp["w_gate"])[0], "w_up": f(inp["w_up"])[0], "w_down": f(inp["w_down"])[0],
    }
    shared.update(_consts())
    x = f(inp["x"]); c = f(inp["c"])
    maps = []
    for i in range(n_cores):
        m = dict(shared)
        m["x"] = np.ascontiguousarray(x[NSEQ * i:NSEQ * (i + 1)])
        m["c"] = np.ascontiguousarray(c[NSEQ * i:NSEQ * (i + 1)])
        maps.append(m)
    return maps


def kernel(**inputs):
    if "nc" not in _CACHE:
        _CACHE["nc"] = build()
    nc = _CACHE["nc"]
    maps = make_in_maps(inputs)
    res = run_bass_kernel_spmd(nc, maps, core_ids=list(range(8)))
    return np.concatenate([r["y"] for r in res.results], axis=0).astype(np.float32)
```
